# Optimizing a Trainium2 kernel written in Bass

```python
import math
import jax, jax.numpy as jnp
from jax import lax
import numpy as np

D_MODEL = 1024
BATCH = 16
SEQ = 4096
DEPTH = 4

LRU_WIDTH = D_MODEL // 4
LRU_BLOCKS = 4
LRU_BLOCK = LRU_WIDTH // LRU_BLOCKS
CONV_WIDTH = 4
LRU_C = 8.0
MLSTM_HEADS = 4
MLSTM_HEAD_DIM = D_MODEL // 8
MLSTM_WIDTH = MLSTM_HEADS * MLSTM_HEAD_DIM
MLSTM_CHUNK = 64
GATE_SOFTCAP = 15.0
ATTN_HEADS = 4
ATTN_HEAD_DIM = D_MODEL // 16
ATTN_WIDTH = ATTN_HEADS * ATTN_HEAD_DIM
Q_LORA_RANK = D_MODEL // 4
KV_LORA_RANK = D_MODEL // 8
IDX_HEADS = 8
IDX_DIM = D_MODEL // 32
INDEX_TOPK = 256
Q_BLOCK = 128
REL_BUCKETS = 32
REL_MAX_EXACT = 16
REL_MAX_DIST = 128
D_FF = 4 * D_MODEL
N_ADA = 6
NORM_EPS = 1e-6
IN_SPLITS = (LRU_WIDTH, LRU_WIDTH,
             MLSTM_WIDTH, MLSTM_WIDTH, MLSTM_WIDTH, MLSTM_WIDTH,
             MLSTM_HEADS, MLSTM_HEADS,
             Q_LORA_RANK, KV_LORA_RANK, IDX_DIM, IDX_HEADS)
N_IN = sum(IN_SPLITS)

kernel_name = 'hymba_lru_mlstm_dsa_block'


def rmsnorm(x, w):
    xf = x.astype(jnp.float32)
    y = xf * lax.rsqrt(jnp.mean(xf * xf, axis=-1, keepdims=True) + NORM_EPS)
    return (y * w.astype(jnp.float32)).astype(x.dtype)


def softcap(z):
    return GATE_SOFTCAP * jnp.tanh(z / GATE_SOFTCAP)


def t5_bucket(dist):
    n = jnp.maximum(dist, 0)
    log_ratio = jnp.log(jnp.maximum(n, 1).astype(jnp.float32) / REL_MAX_EXACT) / math.log(REL_MAX_DIST / REL_MAX_EXACT)
    large = REL_MAX_EXACT + (log_ratio * (REL_BUCKETS - REL_MAX_EXACT)).astype(jnp.int32)
    large = jnp.minimum(large, REL_BUCKETS - 1)
    return jnp.where(n < REL_MAX_EXACT, n, large)


def rg_lru_group(xb, yb, conv_w, conv_b, wa, ba, wx, bx, lam):
    B, S, C = xb.shape
    xc = lax.conv_general_dilated(xb, conv_w[:, None, :], window_strides=(1,),
                                  padding=[(CONV_WIDTH - 1, 0)],
                                  dimension_numbers=('NWC', 'WIO', 'NWC'),
                                  feature_group_count=C) + conv_b
    xblk = xc.reshape(B, S, LRU_BLOCKS, LRU_BLOCK)
    r = jax.nn.sigmoid(jnp.einsum('bsni,nij->bsnj', xblk, wa) + ba).reshape(B, S, C)
    gi = jax.nn.sigmoid(jnp.einsum('bsni,nij->bsnj', xblk, wx) + bx).reshape(B, S, C)
    log_a = -LRU_C * r.astype(jnp.float32) * jax.nn.softplus(-lam.astype(jnp.float32))
    a = jnp.exp(log_a)
    u = jnp.sqrt(-jnp.expm1(2.0 * log_a)) * (gi * xc).astype(jnp.float32)

    def combine(left, right):
        a1, b1 = left
        a2, b2 = right
        return a1 * a2, a2 * b1 + b2

    _, h = lax.associative_scan(combine, (a, u), axis=1)
    return jax.nn.gelu(yb) * h.astype(yb.dtype)


def mlstm_chunkwise(q, k, v, i_g, f_g):
    B, S, H, Dh = q.shape
    nc = S // MLSTM_CHUNK
    q = q.astype(jnp.float32) * (Dh ** -0.5)
    k = k.astype(jnp.float32)
    v = v.astype(jnp.float32)
    logf = jax.nn.log_sigmoid(f_g.astype(jnp.float32))
    ig = i_g.astype(jnp.float32)

    def to_chunks(t):
        return t.reshape(B, nc, MLSTM_CHUNK, H, -1).transpose(1, 0, 3, 2, 4)

    def gate_chunks(t):
        return t.reshape(B, nc, MLSTM_CHUNK, H).transpose(1, 0, 3, 2)

    causal = jnp.tril(jnp.ones((MLSTM_CHUNK, MLSTM_CHUNK), dtype=bool))

    def step(carry, inp):
        C, n, m = carry
        qc, kc, vc, ic, lf = inp
        b = jnp.cumsum(lf, axis=-1)
        D = b[..., :, None] - b[..., None, :] + ic[..., None, :]
        D = jnp.where(causal, D, -jnp.inf)
        inter = b + m[..., None]
        m_row = jnp.maximum(inter, jnp.max(D, axis=-1))
        inter_w = jnp.exp(inter - m_row)
        s = jnp.einsum('bhjd,bhtd->bhjt', qc, kc) * jnp.exp(D - m_row[..., None])
        num = jnp.einsum('bhjt,bhtv->bhjv', s, vc) + inter_w[..., None] * jnp.einsum('bhjk,bhkv->bhjv', qc, C)
        den = jnp.sum(s, axis=-1) + inter_w * jnp.einsum('bhjk,bhk->bhj', qc, n)
        h = num / jnp.maximum(jnp.abs(den), jnp.exp(-m_row))[..., None]
        b_last = b[..., -1]
        w_state = b_last[..., None] - b + ic
        m_new = jnp.maximum(b_last + m, jnp.max(w_state, axis=-1))
        decay = jnp.exp(b_last + m - m_new)
        wexp = jnp.exp(w_state - m_new[..., None])
        C_new = decay[..., None, None] * C + jnp.einsum('bht,bhtk,bhtv->bhkv', wexp, kc, vc)
        n_new = decay[..., None] * n + jnp.einsum('bht,bhtk->bhk', wexp, kc)
        return (C_new, n_new, m_new), h

    init = (jnp.zeros((B, H, Dh, Dh), jnp.float32), jnp.zeros((B, H, Dh), jnp.float32),
            jnp.zeros((B, H), jnp.float32))
    _, hs = lax.scan(step, init, (to_chunks(q), to_chunks(k), to_chunks(v), gate_chunks(ig), gate_chunks(logf)))
    return hs.transpose(1, 0, 3, 2, 4).reshape(B, S, H, Dh)


def mlstm_group(m_q, m_k, m_v, m_o, m_i, m_f, bi, bf, norm_w):
    B, S, _ = m_q.shape
    shp = (B, S, MLSTM_HEADS, MLSTM_HEAD_DIM)
    i_g = softcap(m_i.astype(jnp.float32) + bi)
    f_g = softcap(m_f.astype(jnp.float32) + bf)
    h = mlstm_chunkwise(m_q.reshape(shp), m_k.reshape(shp), m_v.reshape(shp), i_g, f_g)
    h = rmsnorm(h, norm_w.reshape(MLSTM_HEADS, MLSTM_HEAD_DIM))
    return jax.nn.sigmoid(m_o) * h.reshape(B, S, MLSTM_WIDTH).astype(m_o.dtype)


def dsa_group(a_q, a_kv, i_k, i_w, w_q_up, w_qidx_up, w_uk, w_uv, q_norm_w, kv_norm_w, rel_bias):
    B, S, _ = a_q.shape
    q_lat = rmsnorm(a_q, q_norm_w)
    c_kv = rmsnorm(a_kv, kv_norm_w)
    q = jnp.einsum('bsr,rhd->bshd', q_lat, w_q_up)
    q_abs = jnp.einsum('bshd,chd->bshc', q, w_uk) * (ATTN_HEAD_DIM ** -0.5)
    q_idx = jnp.einsum('bsr,rhd->bshd', q_lat, w_qidx_up) * (IDX_DIM ** -0.5)
    w_i = i_w * (IDX_HEADS ** -0.5)
    topk = min(INDEX_TOPK, S // 4)
    nblk = S // Q_BLOCK
    s_pos = jnp.arange(S)

    def block(bidx):
        start = bidx * Q_BLOCK
        t_pos = start + jnp.arange(Q_BLOCK)
        qi = lax.dynamic_slice_in_dim(q_idx, start, Q_BLOCK, axis=1)
        wi = lax.dynamic_slice_in_dim(w_i, start, Q_BLOCK, axis=1)
        qa = lax.dynamic_slice_in_dim(q_abs, start, Q_BLOCK, axis=1)
        score = jnp.einsum('bths,bth->bts', jax.nn.relu(jnp.einsum('bthd,bsd->bths', qi, i_k)), wi).astype(jnp.float32)
        score = jnp.where((s_pos[None, :] <= t_pos[:, None])[None], score, -jnp.inf)
        _, sel = lax.top_k(score, topk)
        valid = sel <= t_pos[None, :, None]
        c_sel = jax.vmap(lambda cb, ib: cb[ib])(c_kv, sel)
        logits = jnp.einsum('bthr,btkr->bthk', qa, c_sel).astype(jnp.float32)
        bias = rel_bias[t5_bucket(t_pos[None, :, None] - sel)]
        logits = logits + bias.transpose(0, 1, 3, 2).astype(jnp.float32)
        logits = jnp.where(valid[:, :, None, :], logits, -jnp.inf)
        p = jax.nn.softmax(logits, axis=-1)
        o_lat = jnp.einsum('bthk,btkr->bthr', p, c_sel)
        return jnp.einsum('bthr,rhd->bthd', o_lat, w_uv).astype(a_q.dtype)

    out = lax.map(block, jnp.arange(nblk))
    return out.transpose(1, 0, 2, 3, 4).reshape(B, S, ATTN_WIDTH)


def hybrid_mixer(h, w_in, conv_w, conv_b, lru_wa, lru_ba, lru_wx, lru_bx, lru_lambda,
                 mlstm_bi, mlstm_bf, w_q_up, w_qidx_up, w_uk, w_uv, q_lat_norm_w, kv_lat_norm_w,
                 rel_bias, group_norm_w, w_o):
    proj = h @ w_in
    split_points = np.cumsum(IN_SPLITS)[:-1].tolist()
    (lru_x, lru_y, m_q, m_k, m_v, m_o, m_i, m_f, a_q, a_kv, i_k, i_w) = jnp.split(proj, split_points, axis=-1)
    g_lru, g_m, g_a = jnp.split(group_norm_w, [LRU_WIDTH, LRU_WIDTH + MLSTM_WIDTH])
    y_lru = rmsnorm(rg_lru_group(lru_x, lru_y, conv_w, conv_b, lru_wa, lru_ba, lru_wx, lru_bx, lru_lambda), g_lru)
    y_m = mlstm_group(m_q, m_k, m_v, m_o, m_i, m_f, mlstm_bi, mlstm_bf, g_m)
    y_a = rmsnorm(dsa_group(a_q, a_kv, i_k, i_w, w_q_up, w_qidx_up, w_uk, w_uv,
                            q_lat_norm_w, kv_lat_norm_w, rel_bias), g_a)
    return jnp.concatenate([y_lru, y_m.astype(y_lru.dtype), y_a], axis=-1) @ w_o


def setup_inputs(seed: int = 0) -> dict:
    key = jax.random.key(seed)
    ks = jax.random.split(key, 32)

    def nrm(k, shape, scale):
        return jax.random.normal(k, shape, jnp.float32) * scale

    u = jax.random.uniform(ks[9], (DEPTH, LRU_WIDTH), jnp.float32, minval=0.9, maxval=0.999)
    s = u ** (1.0 / LRU_C)
    return {
        'x': nrm(ks[0], (BATCH, SEQ, D_MODEL), 1.0),
        'c': nrm(ks[1], (BATCH, D_MODEL), 1.0),
        'w_in': nrm(ks[2], (DEPTH, D_MODEL, N_IN), D_MODEL ** -0.5),
        'conv_w': nrm(ks[3], (DEPTH, CONV_WIDTH, LRU_WIDTH), CONV_WIDTH ** -0.5),
        'conv_b': nrm(ks[4], (DEPTH, LRU_WIDTH), 0.01),
        'lru_wa': nrm(ks[5], (DEPTH, LRU_BLOCKS, LRU_BLOCK, LRU_BLOCK), LRU_BLOCK ** -0.5),
        'lru_ba': nrm(ks[6], (DEPTH, LRU_BLOCKS, LRU_BLOCK), 0.01),
        'lru_wx': nrm(ks[7], (DEPTH, LRU_BLOCKS, LRU_BLOCK, LRU_BLOCK), LRU_BLOCK ** -0.5),
        'lru_bx': nrm(ks[8], (DEPTH, LRU_BLOCKS, LRU_BLOCK), 0.01),
        'lru_lambda': jnp.log(s) - jnp.log1p(-s),
        'mlstm_bi': nrm(ks[10], (DEPTH, MLSTM_HEADS), 0.1),
        'mlstm_bf': jnp.linspace(3.0, 6.0, MLSTM_HEADS, dtype=jnp.float32)[None, :] + nrm(ks[11], (DEPTH, MLSTM_HEADS), 0.1),
        'w_q_up': nrm(ks[12], (DEPTH, Q_LORA_RANK, ATTN_HEADS, ATTN_HEAD_DIM), Q_LORA_RANK ** -0.5),
        'w_qidx_up': nrm(ks[13], (DEPTH, Q_LORA_RANK, IDX_HEADS, IDX_DIM), Q_LORA_RANK ** -0.5),
        'w_uk': nrm(ks[14], (DEPTH, KV_LORA_RANK, ATTN_HEADS, ATTN_HEAD_DIM), KV_LORA_RANK ** -0.5),
        'w_uv': nrm(ks[15], (DEPTH, KV_LORA_RANK, ATTN_HEADS, ATTN_HEAD_DIM), KV_LORA_RANK ** -0.5),
        'q_lat_norm_w': 1.0 + nrm(ks[16], (DEPTH, Q_LORA_RANK), 0.02),
        'kv_lat_norm_w': 1.0 + nrm(ks[17], (DEPTH, KV_LORA_RANK), 0.02),
        'rel_bias': nrm(ks[18], (REL_BUCKETS, ATTN_HEADS), 0.5),
        'group_norm_w': 1.0 + nrm(ks[19], (DEPTH, D_MODEL), 0.02),
        'w_o': nrm(ks[20], (DEPTH, D_MODEL, D_MODEL), D_MODEL ** -0.5),
        'w_ada': nrm(ks[21], (DEPTH, D_MODEL, N_ADA * D_MODEL), 0.5 * D_MODEL ** -0.5),
        'b_ada': nrm(ks[22], (DEPTH, N_ADA * D_MODEL), 0.02),
        'norm1_w': 1.0 + nrm(ks[23], (DEPTH, D_MODEL), 0.02),
        'norm2_w': 1.0 + nrm(ks[24], (DEPTH, D_MODEL), 0.02),
        'w_mlp1': nrm(ks[25], (DEPTH, D_MODEL, D_FF), D_MODEL ** -0.5),
        'w_mlp2': nrm(ks[26], (DEPTH, D_FF, D_MODEL), D_FF ** -0.5),
        'final_norm_w': 1.0 + nrm(ks[27], (D_MODEL,), 0.02),
    }


def reference(x, c, w_in, conv_w, conv_b, lru_wa, lru_ba, lru_wx, lru_bx, lru_lambda,
              mlstm_bi, mlstm_bf, w_q_up, w_qidx_up, w_uk, w_uv, q_lat_norm_w, kv_lat_norm_w,
              rel_bias, group_norm_w, w_o, w_ada, b_ada, norm1_w, norm2_w, w_mlp1, w_mlp2,
              final_norm_w):
    c_act = jax.nn.silu(c)
    for l in range(DEPTH):
        mod = c_act @ w_ada[l] + b_ada[l]
        sh1, sc1, g1, sh2, sc2, g2 = jnp.split(mod[:, None, :], N_ADA, axis=-1)
        h = rmsnorm(x, norm1_w[l]) * (1.0 + sc1) + sh1
        mix = hybrid_mixer(h, w_in[l], conv_w[l], conv_b[l], lru_wa[l], lru_ba[l], lru_wx[l], lru_bx[l],
                           lru_lambda[l], mlstm_bi[l], mlstm_bf[l], w_q_up[l], w_qidx_up[l], w_uk[l], w_uv[l],
                           q_lat_norm_w[l], kv_lat_norm_w[l], rel_bias, group_norm_w[l], w_o[l])
        x = x + g1 * mix
        h = rmsnorm(x, norm2_w[l]) * (1.0 + sc2) + sh2
        x = x + g2 * (jnp.square(jax.nn.relu(h @ w_mlp1[l])) @ w_mlp2[l])
    return rmsnorm(x, final_norm_w)
```

```python
import math
from contextlib import ExitStack

import numpy as np
import concourse.bass as bass
import concourse.mybir as mybir
from concourse.bass_utils import run_bass_kernel_spmd

F32 = mybir.dt.float32
BF16 = mybir.dt.bfloat16
AF = mybir.ActivationFunctionType
ALU = mybir.AluOpType

D = 1024
KD = 8
DEPTH_FULL = 4
N_IN = 2992
O_LX, O_LY, O_Q, O_K, O_V, O_O, O_I, O_F, O_AQ, O_AKV, O_IK, O_IW = (
    0, 256, 512, 1024, 1536, 2048, 2560, 2564, 2568, 2824, 2952, 2984)
NV = 96
TB = 512
NIT = 16
EPS = 1e-6
NEG = -30000.0


class Buf:
    __slots__ = ("w", "r")

    def __init__(self):
        self.w = None
        self.r = {}


class Prog:
    EPOCH = 30000

    def __init__(self, nc, es):
        self.nc = nc
        self.es = es
        self.cur_es = es
        self.engs = {"pe": nc.tensor, "act": nc.scalar, "dve": nc.vector,
                     "pool": nc.gpsimd, "sp": nc.sync}
        self.sems = []
        self.cur = {}
        self.known = {e: {} for e in self.engs}
        for e in ("pe", "act", "dve", "pool"):
            self.cur[e] = [self._newsem(e), 0]
        self.slots = {"sp": [[self._newsem("dsp%d" % i), 0] for i in range(12)],
                      "pool": [[self._newsem("dpl%d" % i), 0] for i in range(8)]}
        self.slot_i = {"sp": 0, "pool": 0}
        self.n_ins = 0

    def _newsem(self, name):
        s = self.es.enter_context(self.nc.semaphore("s%d_%s" % (len(self.sems), name)))
        self.sems.append(s)
        return len(self.sems) - 1

    def _collect(self, eng, reads, writes, deps):
        known = self.known[eng]

        def need(ev):
            if ev is None:
                return
            sid, val, peng = ev
            if peng == eng and eng == "pe":
                return
            if known.get(sid, 0) >= val:
                return
            if deps.get(sid, 0) < val:
                deps[sid] = val

        for b in reads:
            need(b.w)
        for b in writes:
            need(b.w)
            for ev in b.r.values():
                need(ev)

    def _emit_waits(self, eng, deps):
        E = self.engs[eng]
        for sid, val in deps.items():
            E.wait_ge(self.sems[sid], val)
            self.known[eng][sid] = val
            self.n_ins += 1

    def _record(self, ev, key, reads, writes):
        for b in reads:
            b.r[key] = ev
        for b in writes:
            b.w = ev
            b.r = {}

    def op(self, eng, fn, reads=(), writes=()):
        deps = {}
        self._collect(eng, reads, writes, deps)
        self._emit_waits(eng, deps)
        ins = fn(self.engs[eng])
        c = self.cur[eng]
        c[1] += 1
        ins.then_inc(self.sems[c[0]], 1)
        ev = (c[0], c[1], eng)
        self._record(ev, eng, reads, writes)
        if c[1] >= self.EPOCH:
            self.cur[eng] = [self._newsem(eng), 0]
        self.n_ins += 1
        return ins

    def dma(self, q, out, in_, reads=(), writes=(), **kw):
        sl = self.slots[q][self.slot_i[q]]
        self.slot_i[q] = (self.slot_i[q] + 1) % len(self.slots[q])
        deps = {}
        if self.known[q].get(sl[0], 0) < sl[1]:
            deps[sl[0]] = sl[1]
        self._collect(q, reads, writes, deps)
        self._emit_waits(q, deps)
        ins = self.engs[q].dma_start(out=out, in_=in_, **kw)
        sl[1] += 16
        ins.then_inc(self.sems[sl[0]], 16)
        ev = (sl[0], sl[1], "dma")
        self._record(ev, ("dma", sl[0]), reads, writes)
        self.n_ins += 1
        return ins

    def barrier(self):
        evs = []
        for e, c in self.cur.items():
            if c[1] > 0:
                evs.append((c[0], c[1]))
        for q in self.slots:
            for sl in self.slots[q]:
                if sl[1] > 0:
                    evs.append((sl[0], sl[1]))
        for eng in self.engs:
            deps = {}
            for sid, val in evs:
                if self.known[eng].get(sid, 0) < val:
                    deps[sid] = val
            self._emit_waits(eng, deps)

    def final_wait(self, eng="sp"):
        self.barrier()


class TPool:
    def __init__(self, p, name, shape, dtype, n, psum=False):
        self.items = []
        for i in range(n):
            if psum:
                t = p.cur_es.enter_context(p.nc.psum_tensor("%s%d" % (name, i), shape, dtype))
            else:
                t = p.cur_es.enter_context(p.nc.sbuf_tensor("%s%d" % (name, i), shape, dtype))
            self.items.append((t, Buf()))
        self.i = 0

    def next(self):
        it = self.items[self.i]
        self.i = (self.i + 1) % len(self.items)
        return it


def t5_bucket_np(dist):
    n = np.maximum(dist, 0)
    log_ratio = np.log(np.maximum(n, 1).astype(np.float32) / np.float32(16)) / np.float32(math.log(128 / 16))
    large = 16 + (log_ratio * np.float32(16)).astype(np.int32)
    large = np.minimum(large, 31)
    return np.where(n < 16, n, large)


def make_consts():
    c = {}
    i = np.arange(128)
    c["ident"] = np.eye(128, dtype=np.float32)
    c["triu"] = (i[:, None] <= i[None, :]).astype(np.float32)
    c["negts"] = np.where(i[None, :] > i[:, None], np.float32(-1e30), np.float32(0)).astype(np.float32)
    d0 = i[None, :] - i[:, None]
    b0 = t5_bucket_np(d0).astype(np.float32)
    b0 = np.where(d0 >= 0, b0, np.float32(-1.0))
    c["bk0"] = b0.astype(np.float32)
    c["bk1"] = t5_bucket_np(d0 + 128).astype(np.float32)
    sel = np.zeros((8, 3, 128), np.float32)
    for h in range(8):
        sel[h, h // 3, (h % 3) * 32:(h % 3) * 32 + 32] = 1.0
    c["sel8"] = sel
    selh = np.zeros((4, 4, 128), np.float32)
    for h in range(4):
        selh[h, h, :] = 1.0
    c["selh"] = selh
    return c


CONST_SHAPES = {"ident": [128, 128], "triu": [128, 128], "negts": [128, 128], "bk0": [128, 128],
                "bk1": [128, 128], "sel8": [8, 3, 128], "selh": [4, 4, 128]}


def build(S, DEPTH, NSEQ, dbg=False):
    NBLK = S // TB
    NQT = S // 128
    TOPK = min(256, S // 4)
    nc = bass.Bass("TRN2", target_bir_lowering=False)
    es = ExitStack()
    p = Prog(nc, es)

    def din(name, shape):
        return nc.dram_tensor(name, shape, F32, kind="ExternalInput").ap()

    x_d = din("x", [NSEQ, S, D])
    cT_d = din("cT", [128, KD, NSEQ])
    w_in_d = din("w_in", [DEPTH, D, N_IN])
    w_o_d = din("w_o", [DEPTH, D, D])
    w_ada_d = din("w_ada", [DEPTH, D, 6 * D])
    w1_d = din("w_mlp1", [DEPTH, D, 4 * D])
    w2_d = din("w_mlp2", [DEPTH, 4 * D, D])
    wa_d = din("lru_wa", [DEPTH, 4, 64, 64])
    wx_d = din("lru_wx", [DEPTH, 4, 64, 64])
    wqup_d = din("w_q_up", [DEPTH, 256, 256])
    wqidx_d = din("w_qidx_up", [DEPTH, 256, 256])
    wuk_d = din("w_uk", [DEPTH, 128, 256])
    wuv_d = din("w_uv", [DEPTH, 128, 256])
    vec_d = din("vec", [DEPTH, 128, NV])
    gmrow_d = din("gmrow", [DEPTH, 512])
    kvrow_d = din("kvrow", [DEPTH, 128])
    relb_d = din("relb", [128])
    fnw_d = din("fnw", [128, KD])
    cd = {k: din("c_" + k, v) for k, v in CONST_SHAPES.items()}
    out_d = nc.dram_tensor("out", [NSEQ, S, D], F32, kind="ExternalOutput").ap()
    xs_d = nc.dram_tensor("xs", [NSEQ, NBLK, 128, KD, TB], F32).ap()
    dbg_d = {}
    if dbg:
        for nm, shp in (("d_proj", [N_IN, TB]), ("d_ylru", [256, TB]), ("d_ym", [512, TB]),
                        ("d_ya", [256, TB]), ("d_x1", [D, TB]), ("d_x2", [D, TB]),
                        ("d_sc", [128, S]), ("d_thr", [128, 4]), ("d_xb", [D, TB]), ("d_hT", [D, TB]),
                        ("d_mod", [128, 48]), ("d_qaT", [128, 4 * TB]), ("d_ckvT", [128, S]), ("d_olat", [128, 512]),
                        ("d_yasb", [256, TB]), ("d_pT", [128, 512]), ("d_mrow", [1, 4 * TB]), ("d_num", [128, 512]),
                        ("d_den", [128, 512])):
            dbg_d[nm] = nc.dram_tensor(nm, shp, F32, kind="ExternalOutput").ap()

    def sb(name, shape, dt=F32):
        return es.enter_context(nc.sbuf_tensor(name, shape, dt))

    def mm(out, lhsT, rhs, start, stop, reads, writes):
        p.op("pe", lambda E: E.matmul(out, lhsT=lhsT, rhs=rhs, start=start, stop=stop), reads, writes)

    def tr(out, in_, ident, reads, writes):
        p.op("pe", lambda E: E.transpose(out, in_, ident), reads, writes)

    def act(out, in_, func, reads, writes, **kw):
        p.op("act", lambda E: E.activation(out=out, in_=in_, func=func, **kw), reads, writes)

    def ts(out, in0, s1, s2, op0, op1, reads, writes, eng="dve", **kw):
        if op1 is None:
            p.op(eng, lambda E: E.tensor_scalar(out=out, in0=in0, scalar1=s1, scalar2=None, op0=op0, **kw),
                 reads, writes)
        else:
            p.op(eng, lambda E: E.tensor_scalar(out=out, in0=in0, scalar1=s1, scalar2=s2, op0=op0, op1=op1, **kw),
                 reads, writes)

    def tt(out, in0, in1, op, reads, writes, eng="dve"):
        p.op(eng, lambda E: E.tensor_tensor(out=out, in0=in0, in1=in1, op=op), reads, writes)

    def stt(out, in0, scalar, in1, op0, op1, reads, writes):
        p.op("dve", lambda E: E.scalar_tensor_tensor(out=out, in0=in0, scalar=scalar, in1=in1, op0=op0, op1=op1),
             reads, writes)

    def cp(eng, out, in_, reads, writes):
        if eng == "act":
            p.op("act", lambda E: E.copy(out=out, in_=in_), reads, writes)
        else:
            p.op(eng, lambda E: E.tensor_copy(out=out, in_=in_), reads, writes)

    def mset(eng, ap, val, writes):
        p.op(eng, lambda E: E.memset(ap, val), (), writes)

    ident_f = sb("ident_f", [128, 128]); B_const = Buf()
    ident_b = sb("ident_b", [128, 128], BF16)
    triu_b = sb("triu_b", [128, 128], BF16)
    negts = sb("negts", [128, 128])
    sel8 = sb("sel8", [8, 3, 128])
    selh = sb("selh", [4, 4, 128])
    ones_b = sb("ones_b", [128, 128], BF16)
    ones_f = sb("ones_f", [128, 512])
    identx4 = sb("identx4", [128, 4, 128], BF16)
    BT0 = sb("BT0", [128, 4, 128], BF16)
    BT1 = sb("BT1", [128, 4, 128], BF16)
    relb_bc = sb("relb_bc", [128, 128])
    relb_row = sb("relb_row", [1, 128])
    bmax = sb("bmax", [1, 1])
    fnw = sb("fnw_sb", [128, KD])
    modT = sb("modT", [128, DEPTH, 48, NSEQ])
    B_mod = Buf()

    ps_pool = TPool(p, "ps", [128, 512], F32, 6, psum=True)
    acc_pool = TPool(p, "psacc", [128, 512], F32, 2, psum=True)

    p.dma("sp", ident_f[:], cd["ident"][:, :], (), (B_const,))
    p.dma("sp", negts[:], cd["negts"][:, :], (), (B_const,))
    p.dma("sp", sel8[:], cd["sel8"][:, :, :], (), (B_const,))
    p.dma("sp", selh[:], cd["selh"][:, :, :], (), (B_const,))
    p.dma("sp", fnw[:], fnw_d[:, :], (), (B_const,))
    p.dma("sp", relb_bc[:], relb_d.partition_broadcast(128), (), (B_const,))
    p.dma("sp", relb_row[:], relb_d.rearrange("(o n) -> o n", o=1), (), (B_const,))
    p.dma("pool", ident_b[:], cd["ident"][:, :], (), (B_const,))
    p.dma("pool", triu_b[:], cd["triu"][:, :], (), (B_const,))
    mset("dve", ones_b[:], 1.0, (B_const,))
    mset("dve", ones_f[:], 1.0, (B_const,))
    for h in range(4):
        ts(identx4[:, h, :], ident_f[:], 30000.0, None, ALU.mult, None, (B_const,), (B_const,))
    p.op("dve", lambda E: E.tensor_reduce(out=bmax[:], in_=relb_row[:], axis=mybir.AxisListType.X, op=ALU.max,
                                          apply_absolute_value=True), (B_const,), (B_const,))
    with ExitStack() as es2:
        p.cur_es = es2
        bk0 = es2.enter_context(nc.sbuf_tensor("bk0", [128, 128], F32))
        bk1 = es2.enter_context(nc.sbuf_tensor("bk1", [128, 128], F32))
        acc = es2.enter_context(nc.sbuf_tensor("bacc", [128, 2, 4, 128], F32))
        tmpb = es2.enter_context(nc.sbuf_tensor("btmp", [128, 128], F32))
        p.dma("sp", bk0[:], cd["bk0"][:, :], (), (B_const,))
        p.dma("sp", bk1[:], cd["bk1"][:, :], (), (B_const,))
        for h in range(4):
            ts(acc[:, 0, h, :], bk0[:], -1.0, NEG, ALU.is_equal, ALU.mult, (B_const,), (B_const,))
            mset("dve", acc[:, 1, h, :], 0.0, (B_const,))
        for which, bk in ((0, bk0), (1, bk1)):
            for b in range(32):
                for h in range(4):
                    ts(tmpb[:], bk[:], float(b), relb_bc[:, b * 4 + h:b * 4 + h + 1], ALU.is_equal, ALU.mult,
                       (B_const,), (B_const,))
                    tt(acc[:, which, h, :], acc[:, which, h, :], tmpb[:], ALU.add, (B_const,), (B_const,))
        for which in range(2):
            for h in range(4):
                ts(acc[:, which, h, :], acc[:, which, h, :], relb_bc[:, 124 + h:125 + h], None, ALU.subtract, None,
                   (B_const,), (B_const,))
        for h in range(4):
            cp("dve", BT0[:, h, :], acc[:, 0, h, :], (B_const,), (B_const,))
            cp("dve", BT1[:, h, :], acc[:, 1, h, :], (B_const,), (B_const,))
        p.barrier()

    with ExitStack() as es2:
        p.cur_es = es2
        cT = es2.enter_context(nc.sbuf_tensor("cT_sb", [128, KD, NSEQ], F32))
        cTb = es2.enter_context(nc.sbuf_tensor("cTb", [128, KD, NSEQ], F32))
        wad = [es2.enter_context(nc.sbuf_tensor("wad%d" % i, [128, KD, 768], F32)) for i in range(3)]
        wadB = [Buf(), Buf(), Buf()]
        vecs = es2.enter_context(nc.sbuf_tensor("vecs0", [128, DEPTH, NV], F32))
        B_c = Buf()
        p.dma("sp", cT[:], cT_d[:, :, :], (), (B_c,))
        for l in range(DEPTH):
            p.dma("sp", vecs[:, l, :], vec_d[l, :, :], (), (B_c,))
        act(cTb[:], cT[:], AF.Silu, (B_c,), (B_c,))
        it = 0
        for l in range(DEPTH):
            for cc in range(8):
                wt, wb = wad[it % 3], wadB[it % 3]
                it += 1
                for k in range(KD):
                    p.dma("sp", wt[:, k, :], w_ada_d[l, k * 128:(k + 1) * 128, cc * 768:(cc + 1) * 768], (), (wb,))
                for jj in range(6):
                    j = cc * 6 + jj
                    pt, pb = ps_pool.next()
                    for k in range(KD):
                        mm(pt[:, 0:NSEQ], wt[:, k, jj * 128:(jj + 1) * 128], cTb[:, k, :], k == 0, k == KD - 1,
                           (wb, B_c), (pb,))
                    ts(modT[:, l, j, :], pt[:, 0:NSEQ], vecs[:, l, 43 + j:44 + j], None, ALU.add, None,
                       (pb, B_c), (B_mod,))
        for l in range(DEPTH):
            for base in (8, 32):
                ts(modT[:, l, base:base + 8, :], modT[:, l, base:base + 8, :], 1.0, None, ALU.add, None,
                   (B_mod,), (B_mod,))
        p.barrier()

    with ExitStack() as es2:
        p.cur_es = es2
        xin = [es2.enter_context(nc.sbuf_tensor("xin%d" % i, [128, D], F32)) for i in range(3)]
        xinB = [Buf() for _ in range(3)]
        xo = [es2.enter_context(nc.sbuf_tensor("xo%d" % i, [128, KD, 128], F32)) for i in range(3)]
        xoB = [Buf() for _ in range(3)]
        it = 0
        for s in range(NSEQ):
            for tq in range(NQT):
                a, ab = xin[it % 3], xinB[it % 3]
                o, ob = xo[it % 3], xoB[it % 3]
                it += 1
                p.dma("sp", a[:], x_d[s, tq * 128:(tq + 1) * 128, :], (), (ab,))
                for half in range(2):
                    pt, pb = ps_pool.next()
                    for kk in range(4):
                        k = half * 4 + kk
                        tr(pt[:, kk * 128:(kk + 1) * 128], a[:, k * 128:(k + 1) * 128], ident_f[:], (ab, B_const), (pb,))
                    cp("act" if half == 0 else "dve", o[:, half * 4:half * 4 + 4, :],
                       pt[:].rearrange("p (k n) -> p k n", k=4), (pb,), (ob,))
                p.dma("sp", xs_d[s, tq // 4, :, :, (tq % 4) * 128:(tq % 4 + 1) * 128], o[:],
                      (ob,), ())
        p.barrier()

    hT_d = nc.dram_tensor("hT_s", [NSEQ, NBLK, 128, KD, TB], BF16).ap()
    yT_d = nc.dram_tensor("yT_s", [NSEQ, NBLK, 128, KD, TB], BF16).ap()
    aT_d = nc.dram_tensor("aT_s", [NSEQ, NBLK, 128, 32, TB], BF16).ap()
    want_dbg_blk = 1 if NBLK > 1 else 0

    def fm(ap_d, s, c0, n):
        return ap_d[s, c0 // TB, :, :, (c0 % TB):(c0 % TB) + n]

    def rms_bcast(T16, T32, tiles, n_feat, width=TB):
        pss, pssB = ps_pool.next()
        for i, (a, aB) in enumerate(tiles):
            sq, sqB = T16.next()
            act(sq[:, 0:width], a, AF.Square, (aB,), (sqB,))
            mm(pss[:, 0:width], ones_b[:], sq[:, 0:width], i == 0, i == len(tiles) - 1, (sqB, B_const), (pssB,))
        rs, rsB = T32.next()
        act(rs[:, 0:width], pss[:, 0:width], AF.Sqrt, (pssB,), (rsB,), scale=1.0 / n_feat, bias=EPS)
        p.op("dve", lambda E: E.reciprocal(out=rs[:, 0:width], in_=rs[:, 0:width]), (rsB,), (rsB,))
        return rs, rsB

    for l in range(DEPTH):
        with ExitStack() as esA:
            p.cur_es = esA
            vec = esA.enter_context(nc.sbuf_tensor("vec0_%d" % l, [128, NV], F32)); BW = Buf()
            p.dma("sp", vec[:], vec_d[l, :, :], (), (BW,))
            T32 = TPool(p, "p0t32_%d" % l, [128, 512], F32, 4)
            RS0 = TPool(p, "p0rs_%d" % l, [128, 512], F32, 2)
            T16 = TPool(p, "p0t16_%d" % l, [128, 512], BF16, 4)
            xblk_pool = TPool(p, "p0x_%d" % l, [128, KD, TB], F32, 2)
            hT_pool = TPool(p, "p0h_%d" % l, [128, KD, TB], BF16, 2)
            for s in range(NSEQ):
                A1w = esA.enter_context(nc.sbuf_tensor("A1w_%d_%d" % (l, s), [128, 8], F32)); A1wB = Buf()
                tt(A1w[:, 0:8], modT[:, l, 8:16, s], vec[:, 0:8], ALU.mult, (B_mod, BW), (A1wB,))
                for blk in range(NBLK):
                    t0 = blk * TB
                    xb, xbB = xblk_pool.next()
                    p.dma("sp", xb[:], fm(xs_d, s, t0, TB), (), (xbB,))
                    hT, hTB = hT_pool.next()
                    rstd, rstdB = rms_bcast(T16, RS0, [(xb[:, k, :], xbB) for k in range(KD)], D)
                    for k in range(KD):
                        tmp, tmpB = T32.next()
                        tt(tmp[:], xb[:, k, :], rstd[:], ALU.mult, (xbB, rstdB), (tmpB,))
                        act(hT[:, k, :], tmp[:], AF.Identity, (tmpB, A1wB, B_mod), (hTB,),
                            scale=A1w[:, k:k + 1], bias=modT[:, l, k, s:s + 1])
                    p.dma("sp", fm(hT_d, s, t0, TB), hT[:], (hTB,), ())
                    if dbg and l == 0 and s == 0 and blk == want_dbg_blk:
                        p.dma("sp", dbg_d["d_xb"].rearrange("(k p) n -> p k n", p=128), xb[:], (xbB,), ())
                        p.dma("pool", dbg_d["d_hT"].rearrange("(k p) n -> p k n", p=128), hT[:], (hTB,), ())
                        p.dma("sp", dbg_d["d_mod"][:, :], modT[:, 0, :, 0], (B_mod,), ())
            p.barrier()

        def load_w_in(esX, c0, ncol, BW, name):
            w = esX.enter_context(nc.sbuf_tensor("%s_%d" % (name, l), [128, KD, ncol], BF16))
            for k in range(KD):
                p.dma("pool", w[:, k, :], w_in_d[l, k * 128:(k + 1) * 128, c0:c0 + ncol], (), (BW,))
            return w

        def dbg_on(s, blk):
            return dbg and l == 0 and s == 0 and blk == want_dbg_blk

        with ExitStack() as esA:
            p.cur_es = esA
            def sa(name, shape, dt=F32):
                return esA.enter_context(nc.sbuf_tensor("%s_%d" % (name, l), shape, dt))
            BW = Buf()
            wl = load_w_in(esA, 0, 512, BW, "wlru")
            waBD = sa("waBD", [128, 2, 128], BF16)
            wxBD = sa("wxBD", [128, 2, 128], BF16)
            vec = sa("vec1", [128, NV], F32)
            lcl = sa("lcl", [128, 2], F32)
            ltmp = sa("ltmp", [128, 8], F32)
            p.dma("sp", vec[:], vec_d[l, :, :], (), (BW,))
            mset("dve", waBD[:], 0.0, (BW,))
            mset("dve", wxBD[:], 0.0, (BW,))
            for n in range(4):
                r0 = (n % 2) * 64
                p.dma("pool", waBD[r0:r0 + 64, n // 2, r0:r0 + 64], wa_d[l, n, :, :], (), (BW,))
                p.dma("pool", wxBD[r0:r0 + 64, n // 2, r0:r0 + 64], wx_d[l, n, :, :], (), (BW,))
            act(ltmp[:, 0:2], vec[:, 38:40], AF.Exp, (BW,), (BW,), scale=-1.0)
            act(ltmp[:, 2:4], ltmp[:, 0:2], AF.Ln, (BW,), (BW,), bias=1.0)
            ts(ltmp[:, 4:6], ltmp[:, 0:2], -0.25, 1.0 / 3.0, ALU.mult, ALU.add, (BW,), (BW,))
            tt(ltmp[:, 4:6], ltmp[:, 4:6], ltmp[:, 0:2], ALU.mult, (BW,), (BW,))
            ts(ltmp[:, 4:6], ltmp[:, 4:6], -1.0, 0.5, ALU.mult, ALU.add, (BW,), (BW,))
            tt(ltmp[:, 4:6], ltmp[:, 4:6], ltmp[:, 0:2], ALU.mult, (BW,), (BW,))
            ts(ltmp[:, 4:6], ltmp[:, 4:6], -1.0, 1.0, ALU.mult, ALU.add, (BW,), (BW,))
            tt(ltmp[:, 4:6], ltmp[:, 4:6], ltmp[:, 0:2], ALU.mult, (BW,), (BW,))
            ts(ltmp[:, 6:8], ltmp[:, 0:2], 0.1, None, ALU.is_lt, None, (BW,), (BW,))
            tt(ltmp[:, 4:6], ltmp[:, 4:6], ltmp[:, 2:4], ALU.subtract, (BW,), (BW,))
            tt(ltmp[:, 4:6], ltmp[:, 4:6], ltmp[:, 6:8], ALU.mult, (BW,), (BW,))
            tt(ltmp[:, 4:6], ltmp[:, 4:6], ltmp[:, 2:4], ALU.add, (BW,), (BW,))
            ts(lcl[:], ltmp[:, 4:6], -8.0, None, ALU.mult, None, (BW,), (BW,))

            T32 = TPool(p, "p1t32_%d" % l, [128, 512], F32, 24)
            T16 = TPool(p, "p1t16_%d" % l, [128, 512], BF16, 4)
            hT_pool = TPool(p, "p1h_%d" % l, [128, KD, TB], BF16, 2)
            yo_pool = TPool(p, "p1y_%d" % l, [128, 2, TB], BF16, 2)
            lxbuf = sa("lxbuf", [128, 2, 3 + TB], F32); B_lx = Buf()
            lru_h = sa("lru_h", [128, 2], F32); B_lh = Buf()
            for s in range(NSEQ):
                mset("dve", lxbuf[:, :, 0:3], 0.0, (B_lx,))
                mset("dve", lru_h[:], 0.0, (B_lh,))
                for blk in range(NBLK):
                    t0 = blk * TB
                    hT, hTB = hT_pool.next()
                    p.dma("sp", hT[:], fm(hT_d, s, t0, TB), (), (hTB,))
                    yo, yoB = yo_pool.next()

                    def proj(wt, c0, ncol):
                        pt, pb = ps_pool.next()
                        for k in range(KD):
                            mm(pt[0:ncol, :], wt[:, k, c0:c0 + ncol], hT[:, k, :], k == 0, k == KD - 1, (BW, hTB), (pb,))
                        return pt, pb

                    ylru = []
                    for t in range(2):
                        pt, pb = proj(wl, t * 128, 128)
                        cp("act", lxbuf[:, t, 3:3 + TB], pt[:], (pb,), (B_lx,))
                        if dbg_on(s, blk):
                            p.dma("sp", dbg_d["d_proj"][t * 128:(t + 1) * 128, :], lxbuf[:, t, 3:3 + TB], (B_lx,), ())
                        xc, xcB = T32.next()
                        ts(xc[:], lxbuf[:, t, 0:TB], vec[:, 24 + t * 4:25 + t * 4], vec[:, 32 + t:33 + t],
                           ALU.mult, ALU.add, (B_lx, BW), (xcB,))
                        for kk in range(1, 4):
                            stt(xc[:], lxbuf[:, t, kk:kk + TB], vec[:, 24 + t * 4 + kk:25 + t * 4 + kk], xc[:],
                                ALU.mult, ALU.add, (B_lx, BW, xcB), (xcB,))
                        cp("act", lxbuf[:, t, 0:3], lxbuf[:, t, TB:TB + 3], (B_lx,), (B_lx,))
                        xcb, xcbB = T16.next()
                        cp("act", xcb[:], xc[:], (xcB,), (xcbB,))
                        pr, prB = ps_pool.next()
                        mm(pr[:], waBD[:, t, :], xcb[:], True, True, (BW, xcbB), (prB,))
                        pg, pgB = ps_pool.next()
                        mm(pg[:], wxBD[:, t, :], xcb[:], True, True, (BW, xcbB), (pgB,))
                        r, rB = T32.next()
                        act(r[:], pr[:], AF.Sigmoid, (prB, BW), (rB,), bias=vec[:, 34 + t:35 + t])
                        gi, giB = T32.next()
                        act(gi[:], pg[:], AF.Sigmoid, (pgB, BW), (giB,), bias=vec[:, 36 + t:37 + t])
                        a, aB = T32.next()
                        act(a[:], r[:], AF.Exp, (rB, BW), (aB,), scale=lcl[:, t:t + 1])
                        a2, a2B = T32.next()
                        act(a2[:], a[:], AF.Square, (aB,), (a2B,))
                        act(a2[:], a2[:], AF.Sqrt, (a2B,), (a2B,), scale=-1.0, bias=1.0)
                        tt(gi[:], gi[:], xc[:], ALU.mult, (giB, xcB), (giB,))
                        tt(gi[:], gi[:], a2[:], ALU.mult, (giB, a2B), (giB,))
                        hnew, hnB = T32.next()
                        p.op("dve", lambda E: E.tensor_tensor_scan(out=hnew[:], data0=a[:], data1=gi[:],
                                                                   initial=lru_h[:, t:t + 1],
                                                                   op0=ALU.mult, op1=ALU.add),
                             (aB, giB, B_lh), (hnB,))
                        cp("act", lru_h[:, t:t + 1], hnew[:, TB - 1:TB], (hnB,), (B_lh,))
                        py, pyB = proj(wl, 256 + t * 128, 128)
                        yv, yvB = T32.next()
                        cp("act", yv[:], py[:], (pyB,), (yvB,))
                        y2, y2B = T32.next()
                        act(y2[:], yv[:], AF.Square, (yvB,), (y2B,))
                        ts(y2[:], y2[:], 0.044715, 1.0, ALU.mult, ALU.add, (y2B,), (y2B,))
                        tt(y2[:], y2[:], yv[:], ALU.mult, (y2B, yvB), (y2B,))
                        act(y2[:], y2[:], AF.Sigmoid, (y2B,), (y2B,), scale=1.5957691216057308)
                        tt(y2[:], y2[:], yv[:], ALU.mult, (y2B, yvB), (y2B,))
                        tt(hnew[:], hnew[:], y2[:], ALU.mult, (hnB, y2B), (hnB,))
                        ylru.append((hnew, hnB))
                    rs, rsB = rms_bcast(T16, T32, [(ylru[t][0][:], ylru[t][1]) for t in range(2)], 256)
                    for t in range(2):
                        stt(yo[:, t, :], ylru[t][0][:], vec[:, 16 + t:17 + t], rs[:], ALU.mult, ALU.mult,
                            (ylru[t][1], BW, rsB), (yoB,))
                        if dbg_on(s, blk):
                            tmp, tmpB = T32.next()
                            cp("dve", tmp[:], yo[:, t, :], (yoB,), (tmpB,))
                            p.dma("sp", dbg_d["d_ylru"][t * 128:(t + 1) * 128, :], tmp[:], (tmpB,), ())
                    p.dma("sp", yT_d[s, blk, :, 0:2, :], yo[:], (yoB,), ())
            p.barrier()

        with ExitStack() as esA:
            p.cur_es = esA
            def sa(name, shape, dt=F32):
                return esA.enter_context(nc.sbuf_tensor("%s_%d" % (name, l), shape, dt))
            BW = Buf()
            wm = load_w_in(esA, O_Q, O_AQ - O_Q, BW, "wml")
            vec = sa("vec2", [128, NV], F32)
            gm_bc = sa("gm_bc", [128, 512], F32)
            gb15 = sa("gb15", [4, 2], F32)
            p.dma("sp", vec[:], vec_d[l, :, :], (), (BW,))
            p.dma("sp", gm_bc[:], gmrow_d[l, :].partition_broadcast(128), (), (BW,))
            ts(gb15[:], vec[0:4, 91:93], 1.0 / 15.0, None, ALU.mult, None, (BW,), (BW,))
            T32 = TPool(p, "p2t32_%d" % l, [128, 132], F32, 28)
            T16 = TPool(p, "p2t16_%d" % l, [128, 132], BF16, 36)
            hT_pool = TPool(p, "p2h_%d" % l, [128, KD, TB], BF16, 2)
            yo_pool = TPool(p, "p2y_%d" % l, [128, 4, TB], BF16, 2)
            qkT = sa("qkT", [128, 8, TB], BF16); B_qk = Buf()
            oT = sa("oT", [128, 4, TB], BF16); B_oT = Buf()
            vk_tok = sa("vk_tok", [128, 4, 2, 4, 130], BF16); B_vk = Buf()
            Cn = sa("Cn", [128, 4, 130], F32); B_Cnh = [Buf() for _ in range(4)]
            Cnb = sa("Cnb", [128, 4, 130], BF16)
            gates = sa("gates", [4, 4, TB], F32); B_g = Buf()
            tokg = sa("tokg", [128, 4, 12], F32); B_tg = Buf()
            for s in range(NSEQ):
                mset("dve", Cn[:], 0.0, tuple(B_Cnh))
                mset("dve", vk_tok[:, :, 0, :, 128:130], 1.0, (B_vk,))
                for blk in range(NBLK):
                    t0 = blk * TB
                    hT, hTB = hT_pool.next()
                    p.dma("sp", hT[:], fm(hT_d, s, t0, TB), (), (hTB,))
                    yo, yoB = yo_pool.next()

                    def proj(c0, ncol):
                        pt, pb = ps_pool.next()
                        for k in range(KD):
                            mm(pt[0:ncol, :], wm[:, k, c0:c0 + ncol], hT[:, k, :], k == 0, k == KD - 1, (BW, hTB), (pb,))
                        return pt, pb

                    for h in range(4):
                        pt, pb = proj(h * 128, 128)
                        act(qkT[:, h, :], pt[:], AF.Copy, (pb,), (B_qk,), scale=128.0 ** -0.5)
                        pt, pb = proj(512 + h * 128, 128)
                        cp("dve", qkT[:, 4 + h, :], pt[:], (pb,), (B_qk,))
                        pt, pb = proj(1536 + h * 128, 128)
                        act(oT[:, h, :], pt[:], AF.Sigmoid, (pb,), (B_oT,))
                    for c in range(4):
                        for which, col in ((0, 1024), (1, 512)):
                            pt, pb = ps_pool.next()
                            for k in range(KD):
                                mm(pt[:], hT[:, k, c * 128:(c + 1) * 128], wm[:, k, col:col + 512],
                                   k == 0, k == KD - 1, (hTB, BW), (pb,))
                            cp("act" if which == 0 else "dve", vk_tok[:, c, which, :, 0:128],
                               pt[:].rearrange("p (h d) -> p h d", h=4), (pb,), (B_vk,))
                    pi_, piB = proj(2048, 4)
                    act(gates[:, 0, :], pi_[0:4, :], AF.Tanh, (piB, BW), (B_g,), scale=1.0 / 15.0, bias=gb15[:, 0:1])
                    ts(gates[:, 0, :], gates[:, 0, :], 15.0, None, ALU.mult, None, (B_g,), (B_g,))
                    pf_, pfB = proj(2052, 4)
                    act(gates[:, 3, :], pf_[0:4, :], AF.Tanh, (pfB, BW), (B_g,), scale=1.0 / 15.0, bias=gb15[:, 1:2])
                    act(gates[:, 3, :], gates[:, 3, :], AF.Exp, (B_g,), (B_g,), scale=-15.0)
                    act(gates[:, 3, :], gates[:, 3, :], AF.Ln, (B_g,), (B_g,), bias=1.0)
                    ts(gates[:, 1, :], gates[:, 3, :], -1.0, None, ALU.mult, None, (B_g,), (B_g,))
                    for c in range(4):
                        p.op("dve", lambda E: E.tensor_tensor_scan(
                            out=gates[:, 2, c * 128:(c + 1) * 128], data0=ones_f[0:4, c * 128:(c + 1) * 128],
                            data1=gates[:, 1, c * 128:(c + 1) * 128], initial=0.0, op0=ALU.mult, op1=ALU.add),
                             (B_g, B_const), (B_g,))
                    tt(gates[:, 3, :], gates[:, 0, :], gates[:, 2, :], ALU.subtract, (B_g,), (B_g,))
                    for c in range(4):
                        pt, pb = ps_pool.next()
                        mm(pt[:, 0:4], gates[:, 3, c * 128:(c + 1) * 128], ident_f[0:4, 0:4], True, True,
                           (B_g, B_const), (pb,))
                        mm(pt[:, 4:8], gates[:, 2, c * 128:(c + 1) * 128], ident_f[0:4, 0:4], True, True,
                           (B_g, B_const), (pb,))
                        dgl, dglB = T32.next()
                        ts(dgl[0:4, 0:4], ident_f[0:4, 0:4], gates[:, 2, c * 128 + 127:c * 128 + 128], None,
                           ALU.mult, None, (B_g, B_const), (dglB,))
                        mm(pt[:, 8:12], ones_f[0:4, 0:128], dgl[0:4, 0:4], True, True, (dglB, B_const), (pb,))
                        act(tokg[:, c, :], pt[:, 0:12], AF.Exp, (pb,), (B_tg,))
                    for c in range(4):
                        cs = slice(c * 128, (c + 1) * 128)
                        bS, bSB = ps_pool.next()
                        bO = [ps_pool.next(), ps_pool.next()]
                        bU = [ps_pool.next(), ps_pool.next()]
                        bT, bTB = ps_pool.next()
                        bTb = bT[:].bitcast(BF16)
                        H = range(4)
                        vp = [T16.next() for h in H]
                        kb = [T16.next() for h in H]
                        P0 = [T16.next() for h in H]
                        hn = [T16.next() for h in H]
                        sm = [T32.next() for h in H]
                        ho = [T32.next() for h in H]
                        junk = [T32.next() for h in H]

                        def pOv(h):
                            return bO[h // 2][0][:, (h % 2) * 130:(h % 2) * 130 + 130], bO[h // 2][1]

                        def pUv(h):
                            return bU[h // 2][0][:, (h % 2) * 130:(h % 2) * 130 + 130], bU[h // 2][1]

                        for h in H:
                            ts(vp[h][0][:, 0:130], vk_tok[:, c, 0, h, :], tokg[:, c, h:h + 1], None, ALU.mult, None,
                               (B_vk, B_tg), (vp[h][1],))
                            cp("act", kb[h][0][:, 0:128], vk_tok[:, c, 1, h, 0:128], (B_vk,), (kb[h][1],))
                            cp("act", Cnb[:, h, :], Cn[:, h, :], (B_Cnh[h],), (B_Cnh[h],))
                            mm(bS[:, h * 128:(h + 1) * 128], qkT[:, 4 + h, cs], qkT[:, h, cs], True, True, (B_qk,), (bSB,))
                        for h in H:
                            tt(P0[h][0][:, 0:128], bS[:, h * 128:(h + 1) * 128], triu_b[:], ALU.mult,
                               (bSB, B_const), (P0[h][1],))
                        for h in H:
                            po, poB = pOv(h)
                            mm(po, P0[h][0][:, 0:128], vp[h][0][:, 0:130], True, False, (P0[h][1], vp[h][1]), (poB,))
                            mm(po, qkT[:, h, cs], Cnb[:, h, :], False, True, (B_qk, B_Cnh[h]), (poB,))
                            pu, puB = pUv(h)
                            mm(pu, kb[h][0][:, 0:128], vp[h][0][:, 0:130], True, True, (kb[h][1], vp[h][1]), (puB,))
                        for h in H:
                            pu, puB = pUv(h)
                            tt(Cn[:, h, :], Cn[:, h, :], pu, ALU.add, (B_Cnh[h], puB), (B_Cnh[h],))
                            ts(Cn[:, h, :], Cn[:, h, :], tokg[:, c, 8 + h:9 + h], None, ALU.mult, None,
                               (B_Cnh[h], B_tg), (B_Cnh[h],))
                        for h in H:
                            po, poB = pOv(h)
                            act(sm[h][0][:, 0:1], po[:, 128:129], AF.Abs, (poB, B_tg), (sm[h][1],),
                                scale=tokg[:, c, 4 + h:5 + h])
                        for h in H:
                            po, poB = pOv(h)
                            smt, smB = sm[h]
                            ts(smt[:, 0:1], smt[:, 0:1], 1.0, None, ALU.max, None, (smB,), (smB,))
                            p.op("dve", lambda E: E.reciprocal(out=smt[:, 1:2], in_=smt[:, 0:1]), (smB,), (smB,))
                            tt(smt[:, 2:3], smt[:, 1:2], tokg[:, c, 4 + h:5 + h], ALU.mult, (smB, B_tg), (smB,))
                            ts(ho[h][0][:, 0:128], po[:, 0:128], smt[:, 2:3], None, ALU.mult, None, (poB, smB), (ho[h][1],))
                        for h in H:
                            smt, smB = sm[h]
                            act(junk[h][0][:, 0:128], ho[h][0][:, 0:128], AF.Square, (ho[h][1],), (junk[h][1], smB),
                                accum_out=smt[:, 3:4])
                            act(smt[:, 4:5], smt[:, 3:4], AF.Sqrt, (smB,), (smB,), scale=1.0 / 128, bias=EPS)
                        for h in H:
                            smt, smB = sm[h]
                            p.op("dve", lambda E: E.reciprocal(out=smt[:, 5:6], in_=smt[:, 4:5]), (smB,), (smB,))
                            stt(hn[h][0][:, 0:128], ho[h][0][:, 0:128], smt[:, 5:6], gm_bc[:, h * 128:(h + 1) * 128],
                                ALU.mult, ALU.mult, (ho[h][1], smB, BW), (hn[h][1],))
                        for h in H:
                            tr(bTb[:, h * 128:(h + 1) * 128], hn[h][0][:, 0:128], ident_b[:], (hn[h][1], B_const), (bTB,))
                        for h in H:
                            tt(yo[:, h, cs], bTb[:, h * 128:(h + 1) * 128], oT[:, h, cs], ALU.mult, (bTB, B_oT), (yoB,))
                    if dbg_on(s, blk):
                        for h in range(4):
                            for half in range(4):
                                tmp, tmpB = T32.next()
                                cp("dve", tmp[:, 0:128], yo[:, h, half * 128:(half + 1) * 128], (yoB,), (tmpB,))
                                p.dma("sp", dbg_d["d_ym"][h * 128:(h + 1) * 128, half * 128:(half + 1) * 128],
                                      tmp[:, 0:128], (tmpB,), ())
                    p.dma("sp", yT_d[s, blk, :, 2:6, :], yo[:], (yoB,), ())
            p.barrier()

        with ExitStack() as esA:
            p.cur_es = esA
            def sa(name, shape, dt=F32):
                return esA.enter_context(nc.sbuf_tensor("%s_%d" % (name, l), shape, dt))
            BW = Buf()
            wd = load_w_in(esA, O_AQ, N_IN - O_AQ, BW, "wdsa")
            w_ik4 = sa("w_ik4", [128, KD, 128], BF16)
            wqup = sa("wqup", [128, 2, 256], BF16)
            wqidx = sa("wqidx", [128, 2, 256], BF16)
            wqidx3 = sa("wqidx3", [128, 2, 3, 128], BF16)
            wukT = sa("wukT", [128, 2, 128], BF16)
            wuv_pad = sa("wuvp", [128, 4, 128], BF16)
            wuv_tmp = sa("wuvt", [128, 256], BF16)
            wuk_tmp = sa("wukt", [128, 256], F32)
            vec = sa("vec3", [128, NV], F32)
            kvrow = sa("kvrow", [1, 128], F32)
            cmax2 = sa("cmax2", [1, 1], F32)
            for k in range(2):
                p.dma("pool", wqup[:, k, :], wqup_d[l, k * 128:(k + 1) * 128, :], (), (BW,))
                p.dma("pool", wqidx[:, k, :], wqidx_d[l, k * 128:(k + 1) * 128, :], (), (BW,))
            p.dma("pool", wuv_tmp[:], wuv_d[l, :, :], (), (BW,))
            p.dma("sp", wuk_tmp[:], wuk_d[l, :, :], (), (BW,))
            p.dma("sp", vec[:], vec_d[l, :, :], (), (BW,))
            p.dma("sp", kvrow[:], kvrow_d[l:l + 1, :], (), (BW,))
            mset("dve", wuv_pad[:], 0.0, (BW,))
            mset("dve", wqidx3[:], 0.0, (BW,))
            for h in range(8):
                cp("act", wqidx3[:, :, h // 3, (h % 3) * 32:(h % 3) * 32 + 32], wqidx[:, :, h * 32:(h + 1) * 32],
                   (BW,), (BW,))
            for j in range(4):
                cp("act", w_ik4[:, :, j * 32:(j + 1) * 32], wd[:, :, 384:416], (BW,), (BW,))
            for h in range(4):
                c0 = (h % 2) * 64
                cp("act", wuv_pad[:, h, c0:c0 + 64], wuv_tmp[:, h * 64:(h + 1) * 64], (BW,), (BW,))
            for hp in range(2):
                pt, pb = ps_pool.next()
                tr(pt[:, 0:128], wuk_tmp[:, hp * 128:(hp + 1) * 128], ident_f[:], (BW, B_const), (pb,))
                ts(wukT[:, hp, :], pt[:, 0:128], 0.125, None, ALU.mult, None, (pb,), (BW,))
            p.op("dve", lambda E: E.tensor_reduce(out=cmax2[:], in_=kvrow[:], axis=mybir.AxisListType.X, op=ALU.max,
                                                  apply_absolute_value=True), (BW,), (BW,))
            tt(cmax2[:], cmax2[:], cmax2[:], ALU.mult, (BW,), (BW,))
            ts(cmax2[:], cmax2[:], 128.0, None, ALU.mult, None, (BW,), (BW,))

            T32 = TPool(p, "p3t32_%d" % l, [128, 512], F32, 6)
            T16 = TPool(p, "p3t16_%d" % l, [128, 512], BF16, 4)
            hT_pool = TPool(p, "p3h_%d" % l, [128, KD, TB], BF16, 1)
            yo_pool = TPool(p, "p3y_%d" % l, [128, 2, TB], BF16, 2)
            ckvT = sa("ckvT", [128, S], BF16); B_ckvT = [Buf() for _ in range(NBLK)]
            ckv_tok = sa("ckv_tok", [128, NQT, 128], BF16); B_ckt = [Buf() for _ in range(NBLK)]
            kidx4 = sa("kidx4", [128, S], BF16); B_kidx = [Buf() for _ in range(NBLK)]
            qlatT = sa("qlatT", [128, 2, TB], BF16); B_ql = Buf()
            qsT = sa("qsT", [128, 3, TB], BF16); B_qs = Buf()
            qTsb = sa("qTsb", [128, 2, TB], BF16); B_qT = Buf()
            qaT2 = [sa("qaT%d" % i, [128, 4, TB], BF16) for i in range(2)]; B_qa2 = [Buf(), Buf()]
            iwT = sa("iwT", [8, 2, TB], F32); B_iw = Buf()
            sgn_tok = sa("sgn_tok", [128, 4, 8], F32); B_sg = Buf()
            dg2 = [sa("dg%d" % i, [128, 8, 128], BF16) for i in range(2)]; B_dg2 = [Buf(), Buf()]
            mrow2 = [sa("mrow%d" % i, [1, 4, TB], BF16) for i in range(2)]; B_mr2 = [Buf(), Buf()]
            mrowf = sa("mrowf", [1, TB], F32); B_mrf = Buf()
            sc2 = [sa("sc%d" % i, [128, S], F32) for i in range(2)]; B_sc2 = [Buf(), Buf()]
            negm2 = [sa("negm%d" % i, [128, S], BF16) for i in range(2)]; B_nm2 = [Buf(), Buf()]
            rh_pool = TPool(p, "rh_%d" % l, [128, 512], BF16, 10)
            pT_pool = TPool(p, "pT_%d" % l, [128, 512], BF16, 3)
            bis2 = [sa("bis%d" % i, [128, 8], F32) for i in range(2)]; B_bis2 = [Buf(), Buf()]
            dcols2 = [sa("dcols%d" % i, [128, NIT + 1], F32) for i in range(2)]
            pow2 = sa("pow2", [128, NIT + 1], F32)
            olat = sa("olat", [128, 4, 128], BF16); B_ol = Buf()
            ya2 = [sa("ya_sb%d" % i, [128, 2, TB], F32) for i in range(2)]; B_ya2 = [Buf(), Buf()]
            nd2 = [sa("nd%d" % i, [128, 2, 512], F32) for i in range(2)]; B_nd2 = [Buf(), Buf()]
            for k in range(NIT + 1):
                mset("dve", pow2[:, k:k + 1], 0.5 ** (k + 1), (BW,))

            def prep(s, blk):
                t0 = blk * TB
                par = blk % 2
                hT, hTB = hT_pool.next()
                p.dma("sp", hT[:], fm(hT_d, s, t0, TB), (), (hTB,))

                def proj(wt, c0, ncol):
                    pt, pb = ps_pool.next()
                    for k in range(KD):
                        mm(pt[0:ncol, :], wt[:, k, c0:c0 + ncol], hT[:, k, :], k == 0, k == KD - 1, (BW, hTB), (pb,))
                    return pt, pb

                aq = []
                for t in range(2):
                    pt, pb = proj(wd, t * 128, 128)
                    av, avB = T32.next()
                    cp("act", av[:], pt[:], (pb,), (avB,))
                    aq.append((av, avB))
                rs, rsB = rms_bcast(T16, T32, [(aq[t][0][:], aq[t][1]) for t in range(2)], 256)
                for t in range(2):
                    stt(qlatT[:, t, :], aq[t][0][:], vec[:, 40 + t:41 + t], rs[:], ALU.mult, ALU.mult,
                        (aq[t][1], BW, rsB), (B_ql,))
                pt, pb = proj(wd, 256, 128)
                av, avB = T32.next()
                cp("act", av[:], pt[:], (pb,), (avB,))
                rs, rsB = rms_bcast(T16, T32, [(av[:], avB)], 128)
                stt(ckvT[:, t0:t0 + TB], av[:], vec[:, 42:43], rs[:], ALU.mult, ALU.mult,
                    (avB, BW, rsB), (B_ckvT[blk],))
                for c in range(4):
                    pT_, pTB = ps_pool.next()
                    ptb = pT_[:].bitcast(BF16)
                    tr(ptb[:, 0:128], ckvT[:, t0 + c * 128:t0 + (c + 1) * 128], ident_b[:], (B_ckvT[blk], B_const), (pTB,))
                    cp("act", ckv_tok[:, blk * 4 + c, :], ptb[:, 0:128], (pTB,), (B_ckt[blk],))
                pt, pb = proj(w_ik4, 0, 128)
                cp("act", kidx4[:, t0:t0 + TB], pt[:], (pb,), (B_kidx[blk],))
                pw, pwB = proj(wd, 416, 8)
                ts(iwT[:, 0, :], pw[0:8, :], 8.0 ** -0.5, None, ALU.mult, None, (pwB,), (B_iw,))
                act(iwT[:, 1, :], iwT[:, 0, :], AF.Abs, (B_iw,), (B_iw,))
                for c in range(4):
                    pt, pb = ps_pool.next()
                    mm(pt[:, 0:8], iwT[:, 0, c * 128:(c + 1) * 128], ident_f[0:8, 0:8], True, True,
                       (B_iw, B_const), (pb,))
                    act(sgn_tok[:, c, :], pt[:, 0:8], AF.Sign, (pb,), (B_sg,))
                for t in range(3):
                    pq, pqB = ps_pool.next()
                    for k in range(2):
                        mm(pq[:], wqidx3[:, k, t, :], qlatT[:, k, :], k == 0, k == 1, (BW, B_ql), (pqB,))
                    pwb, pwbB = ps_pool.next()
                    mm(pwb[:], sel8[:, t, :], iwT[:, 1, :], True, True, (B_const, B_iw), (pwbB,))
                    wab, wabB = T32.next()
                    act(wab[:], pwb[:], AF.Copy, (pwbB,), (wabB,), scale=32.0 ** -0.5)
                    tt(qsT[:, t, :], pq[:], wab[:], ALU.mult, (pqB, wabB), (B_qs,))
                for t in range(2):
                    pq, pqB = ps_pool.next()
                    for k in range(2):
                        mm(pq[:], wqup[:, k, t * 128:(t + 1) * 128], qlatT[:, k, :], k == 0, k == 1, (BW, B_ql), (pqB,))
                    cp("act", qTsb[:, t, :], pq[:], (pqB,), (B_qT,))
                qaT, B_qa, mrow, B_mr = qaT2[par], B_qa2[par], mrow2[par], B_mr2[par]
                for h in range(4):
                    r0 = (h % 2) * 64
                    pa, paB = ps_pool.next()
                    mm(pa[:], wukT[r0:r0 + 64, h // 2, :], qTsb[r0:r0 + 64, h // 2, :], True, True, (BW, B_qT), (paB,))
                    cp("act", qaT[:, h, :], pa[:], (paB,), (B_qa,))
                    sq, sqB = T16.next()
                    act(sq[:], pa[:], AF.Square, (paB,), (sqB,))
                    qn2, qn2B = ps_pool.next()
                    mm(qn2[0:1, :], ones_b[:, 0:1], sq[:], True, True, (sqB, B_const), (qn2B,))
                    ts(mrowf[:], qn2[0:1, :], cmax2[:, 0:1], None, ALU.mult, None, (qn2B, BW), (B_mrf,))
                    act(mrowf[:], mrowf[:], AF.Sqrt, (B_mrf,), (B_mrf,))
                    ts(mrowf[:], mrowf[:], bmax[:, 0:1], -1.0, ALU.add, ALU.mult, (B_mrf, B_const), (B_mrf,))
                    ts(mrow[:, h, :], mrowf[:], relb_row[:, 124 + h:125 + h], None, ALU.add, None,
                       (B_mrf, B_const), (B_mr,))

            def geom(u):
                s, blk, c = u
                qi = blk * 4 + c
                nk = (qi + 1) * 128
                return qi, nk, (nk + 511) // 512, slice(c * 128, (c + 1) * 128), nk > TOPK

            def idx(u, ui):
                s, blk, c = u
                qi, nk, nch, qcs, use_topk = geom(u)
                if not use_topk:
                    return
                dg, B_dg = dg2[ui % 2], B_dg2[ui % 2]
                sc, B_sc = sc2[ui % 2], B_sc2[ui % 2]
                for hh in range(8):
                    ts(dg[:, hh, :], ident_f[:], sgn_tok[:, c, hh:hh + 1], None, ALU.mult, None,
                       (B_const, B_sg), (B_dg,), eng="pool")
                for ch in range(nch):
                    w = min(512, nk - ch * 512)
                    ks = slice(ch * 512, ch * 512 + w)
                    rhs_ = []
                    for hh in range(8):
                        r0 = (hh % 3) * 32
                        pi2, pi2B = ps_pool.next()
                        mm(pi2[:, 0:w], qsT[r0:r0 + 32, hh // 3, qcs], kidx4[r0:r0 + 32, ks], True, True,
                           (B_qs, B_kidx[ch]), (pi2B,))
                        rh, rhB = rh_pool.next()
                        act(rh[:, 0:w], pi2[:, 0:w], AF.Relu, (pi2B,), (rhB,))
                        rhs_.append((rh, rhB))
                    pacc, paccB = ps_pool.next()
                    for hh in range(8):
                        mm(pacc[:, 0:w], dg[:, hh, :], rhs_[hh][0][:, 0:w], hh == 0, hh == 7,
                           (B_dg, rhs_[hh][1]), (paccB,))
                    cp("act", sc[:, ks], pacc[:, 0:w], (paccB,), (B_sc,))

            def bisect(u, ui):
                s, blk, c = u
                qi, nk, nch, qcs, use_topk = geom(u)
                if not use_topk:
                    return
                negm, B_nm = negm2[ui % 2], B_nm2[ui % 2]
                sc, B_sc = sc2[ui % 2], B_sc2[ui % 2]
                bis, B_bis, dcols = bis2[ui % 2], B_bis2[ui % 2], dcols2[ui % 2]
                on_act = False
                p.op("dve", lambda E: E.tensor_reduce(out=bis[:, 0:1], in_=sc[:, 0:nk], axis=mybir.AxisListType.X,
                                                      op=ALU.min), (B_sc,), (B_bis,))
                tt(sc[:, nk - 128:nk], sc[:, nk - 128:nk], negts[:], ALU.add, (B_sc, B_const), (B_sc,))
                p.op("dve", lambda E: E.tensor_reduce(out=bis[:, 1:2], in_=sc[:, 0:nk], axis=mybir.AxisListType.X,
                                                      op=ALU.max), (B_sc,), (B_bis,))
                tt(bis[:, 2:3], bis[:, 1:2], bis[:, 0:1], ALU.subtract, (B_bis,), (B_bis,))
                ts(dcols[:], pow2[:], bis[:, 2:3], None, ALU.mult, None, (B_bis, BW), (B_bis,))
                tt(bis[:, 3:4], bis[:, 0:1], dcols[:, 0:1], ALU.add, (B_bis,), (B_bis,))
                if not on_act:
                    for k in range(NIT):
                        ts(negm[:, 0:nk], sc[:, 0:nk], bis[:, 3:4], 0.0, ALU.is_ge, ALU.add,
                           (B_sc, B_bis), (B_nm, B_bis), accum_out=bis[:, 4:5])
                        stt(bis[:, 5:6], bis[:, 4:5], float(TOPK) - 0.5, dcols[:, k:k + 1], ALU.is_ge, ALU.mult,
                            (B_bis,), (B_bis,))
                        stt(bis[:, 3:4], bis[:, 3:4], dcols[:, k + 1:k + 2], bis[:, 5:6], ALU.subtract, ALU.add,
                            (B_bis,), (B_bis,))
                else:
                    ts(bis[:, 7:8], bis[:, 3:4], -1.0, None, ALU.mult, None, (B_bis,), (B_bis,))
                    ts(dcols[:], dcols[:], -1.0, None, ALU.mult, None, (B_bis,), (B_bis,))
                    for k in range(NIT):
                        act(negm[:, 0:nk], sc[:, 0:nk], AF.Sign, (B_sc, B_bis), (B_nm, B_bis),
                            bias=bis[:, 7:8], scale=1.0, accum_out=bis[:, 4:5])
                        act(bis[:, 5:6], bis[:, 4:5], AF.Sign, (B_bis,), (B_bis,), scale=1.0,
                            bias=float(nk - 2 * TOPK + 1))
                        act(bis[:, 7:8], bis[:, 5:6], AF.Identity, (B_bis,), (B_bis,),
                            scale=dcols[:, k + 1:k + 2], bias=bis[:, 7:8])
                    ts(bis[:, 3:4], bis[:, 7:8], -1.0, None, ALU.mult, None, (B_bis,), (B_bis,))
                    ts(dcols[:, NIT:NIT + 1], dcols[:, NIT:NIT + 1], -1.0, None, ALU.mult, None, (B_bis,), (B_bis,))
                tt(bis[:, 6:7], bis[:, 3:4], dcols[:, NIT:NIT + 1], ALU.subtract, (B_bis,), (B_bis,))
                ts(negm[:, 0:nk], sc[:, 0:nk], bis[:, 6:7], -1.0, ALU.is_ge, ALU.add, (B_sc, B_bis), (B_nm,))
                if dbg and l == 0 and s == 0 and c == 3 and blk == NBLK - 1:
                    p.dma("sp", dbg_d["d_sc"][:, 0:nk], sc[:, 0:nk], (B_sc,), ())
                    p.dma("sp", dbg_d["d_thr"][:, 0:4], bis[:, 3:7], (B_bis,), ())

            def att(u, ui):
                s, blk, c = u
                qi, nk, nch, qcs, use_topk = geom(u)
                par = blk % 2
                qaT, B_qa, mrow, B_mr = qaT2[par], B_qa2[par], mrow2[par], B_mr2[par]
                negm, B_nm = negm2[ui % 2], B_nm2[ui % 2]
                pnum, pnumB = acc_pool.next()
                pden, pdenB = acc_pool.next()

                def logits(sj):
                    ss = slice(sj * 128, (sj + 1) * 128)
                    near = (qi - sj) <= 1
                    pl, plB = ps_pool.next()
                    pl3 = pl[:].rearrange("p (h t) -> p h t", h=4)
                    mm(pl3, ckvT[:, ss], qaT[:, :, qcs], True, False, (B_ckvT[sj // 4], B_qa), (plB,))
                    if near:
                        BT = BT0 if qi == sj else BT1
                        mm(pl[:], ident_b[:], BT[:].rearrange("p h t -> p (h t)"), False, False, (B_const,), (plB,))
                    if use_topk:
                        mm(pl[:], negm[:, ss], identx4[:].rearrange("p h t -> p (h t)"), False, False,
                           (B_nm, B_const), (plB,))
                    mm(pl3, ones_b[0:1, 0:128], mrow[:, :, qcs], False, True, (B_const, B_mr), (plB,))
                    return pl, plB

                cur = logits(0)
                for sj in range(qi + 1):
                    pl, plB = cur
                    pT, pTB2 = pT_pool.next()
                    act(pT[:], pl[:], AF.Exp, (plB,), (pTB2,))
                    if sj + 1 <= qi:
                        cur = logits(sj + 1)
                    mm(pnum[:], ckv_tok[:, sj, :], pT[:], sj == 0, sj == qi, (B_ckt[sj // 4], pTB2), (pnumB,))
                    mm(pden[:], ones_b[:], pT[:], sj == 0, sj == qi, (B_const, pTB2), (pdenB,))
                nd, ndB = nd2[ui % 2], B_nd2[ui % 2]
                cp("act", nd[:, 0, :], pnum[:], (pnumB,), (ndB,))
                act(nd[:, 1, :], pden[:], AF.Ln, (pdenB,), (ndB,))
                act(nd[:, 1, :], nd[:, 1, :], AF.Exp, (ndB,), (ndB,), scale=-1.0)

            def epi(u, ui):
                s, blk, c = u
                qi, nk, nch, qcs, use_topk = geom(u)
                par = blk % 2
                ya_sb, B_ya = ya2[par], B_ya2[par]
                nd, ndB = nd2[ui % 2], B_nd2[ui % 2]
                tt(olat[:].rearrange("p h t -> p (h t)"), nd[:, 0, :], nd[:, 1, :], ALU.mult, (ndB,), (B_ol,))
                for t in range(2):
                    py_, pyB_ = ps_pool.next()
                    for hh in range(2):
                        h = t * 2 + hh
                        mm(py_[:, 0:128], wuv_pad[:, h, :], olat[:, h, :], hh == 0, hh == 1, (BW, B_ol), (pyB_,))
                    cp("act", ya_sb[:, t, qcs], py_[:, 0:128], (pyB_,), (B_ya,))
                if c == 3:
                    finish(s, blk)

            def finish(s, blk):
                t0 = blk * TB
                par = blk % 2
                ya_sb, B_ya = ya2[par], B_ya2[par]
                yo, yoB = yo_pool.next()
                rs, rsB = rms_bcast(T16, T32, [(ya_sb[:, t, :], B_ya) for t in range(2)], 256)
                for t in range(2):
                    stt(yo[:, t, :], ya_sb[:, t, :], vec[:, 22 + t:23 + t], rs[:], ALU.mult, ALU.mult,
                        (B_ya, BW, rsB), (yoB,))
                    if dbg_on(s, blk):
                        tmp, tmpB = T32.next()
                        cp("dve", tmp[:], yo[:, t, :], (yoB,), (tmpB,))
                        p.dma("sp", dbg_d["d_ya"][t * 128:(t + 1) * 128, :], tmp[:], (tmpB,), ())
                p.dma("sp", yT_d[s, blk, :, 6:8, :], yo[:], (yoB,), ())

            units = [(s, blk, c) for s in range(NSEQ) for blk in range(NBLK) for c in range(4)]
            nu = len(units)
            prep(units[0][0], units[0][1])
            idx(units[0], 0)
            for ui, u in enumerate(units):
                if ui + 1 < nu:
                    nx = units[ui + 1]
                    if nx[2] == 0:
                        prep(nx[0], nx[1])
                    idx(nx, ui + 1)
                bisect(u, ui)
                if ui > 0:
                    att(units[ui - 1], ui - 1)
                if ui > 1:
                    epi(units[ui - 2], ui - 2)
            att(units[-1], nu - 1)
            if nu > 1:
                epi(units[-2], nu - 2)
            epi(units[-1], nu - 1)
            p.barrier()

        with ExitStack() as esA:
            p.cur_es = esA
            BW = Buf()
            w_o_sb = esA.enter_context(nc.sbuf_tensor("w_o_%d" % l, [128, KD, D], BF16))
            for k in range(KD):
                p.dma("pool", w_o_sb[:, k, :], w_o_d[l, k * 128:(k + 1) * 128, :], (), (BW,))
            xblk_pool = TPool(p, "p4x_%d" % l, [128, KD, TB], F32, 2)
            yT_pool = TPool(p, "p4y_%d" % l, [128, KD, TB], BF16, 2)
            for s in range(NSEQ):
                for blk in range(NBLK):
                    t0 = blk * TB
                    xb, xbB = xblk_pool.next()
                    p.dma("sp", xb[:], fm(xs_d, s, t0, TB), (), (xbB,))
                    yT, yTB = yT_pool.next()
                    p.dma("sp", yT[:], fm(yT_d, s, t0, TB), (), (yTB,))
                    for m in range(KD):
                        po, poB = ps_pool.next()
                        for k in range(KD):
                            mm(po[:], w_o_sb[:, k, m * 128:(m + 1) * 128], yT[:, k, :], k == 0, k == KD - 1,
                               (BW, yTB), (poB,))
                        stt(xb[:, m, :], po[:], modT[:, l, 16 + m, s:s + 1], xb[:, m, :], ALU.mult, ALU.add,
                            (poB, B_mod, xbB), (xbB,))
                    p.dma("sp", fm(xs_d, s, t0, TB), xb[:], (xbB,), ())
                    if dbg_on(s, blk):
                        p.dma("sp", dbg_d["d_x1"].rearrange("(k p) n -> p k n", p=128), xb[:], (xbB,), ())
            p.barrier()

        with ExitStack() as esB:
            p.cur_es = esB
            def sbb(name, shape, dt=F32):
                return esB.enter_context(nc.sbuf_tensor("%s_%d" % (name, l), shape, dt))

            BW = Buf()
            w1 = sbb("w1", [128, KD, 4 * D], BF16)
            vec = sbb("vecB", [128, NV], F32)
            p.dma("sp", vec[:], vec_d[l, :, :], (), (BW,))
            for k in range(KD):
                p.dma("pool", w1[:, k, :], w1_d[l, k * 128:(k + 1) * 128, :], (), (BW,))
            xq_pool = TPool(p, "xq_%d" % l, [128, KD, TB], F32, 1)
            h2_pool = TPool(p, "h2_%d" % l, [128, KD, TB], BF16, 2)
            aT_pool = TPool(p, "aT_%d" % l, [128, 8, TB], BF16, 4)
            M32 = TPool(p, "m32_%d" % l, [128, TB], F32, 4)
            M16 = TPool(p, "m16_%d" % l, [128, TB], BF16, 4)
            RS5 = TPool(p, "p5rs_%d" % l, [128, TB], F32, 2)
            for s in range(NSEQ):
                A2s = sbb("A2s_%d" % s, [128, 8], F32); A2B = Buf()
                tt(A2s[:], modT[:, l, 32:40, s], vec[:, 8:16], ALU.mult, (B_mod, BW), (A2B,))
                for blk in range(NBLK):
                    t0 = blk * TB
                    xb, xbB = xq_pool.next()
                    p.dma("sp", xb[:], fm(xs_d, s, t0, TB), (), (xbB,))
                    rstd, rstdB = rms_bcast(M16, RS5, [(xb[:, k, :], xbB) for k in range(KD)], D)
                    h2, h2B = h2_pool.next()
                    for k in range(KD):
                        tmp, tmpB = M32.next()
                        tt(tmp[:], xb[:, k, :], rstd[:], ALU.mult, (xbB, rstdB), (tmpB,))
                        act(h2[:, k, :], tmp[:], AF.Identity, (tmpB, A2B, B_mod), (h2B,),
                            scale=A2s[:, k:k + 1], bias=modT[:, l, 24 + k, s:s + 1])
                    for q in range(4):
                        aT, aTB = aT_pool.next()
                        for mm_ in range(8):
                            m = q * 8 + mm_
                            pm, pmB = ps_pool.next()
                            for k in range(KD):
                                mm(pm[:], w1[:, k, m * 128:(m + 1) * 128], h2[:, k, :], k == 0, k == KD - 1,
                                   (BW, h2B), (pmB,))
                            rl, rlB = M32.next()
                            act(rl[:], pm[:], AF.Relu, (pmB,), (rlB,))
                            tt(aT[:, mm_, :], rl[:], rl[:], ALU.mult, (rlB,), (aTB,))
                        p.dma("sp", aT_d[s, blk, :, q * 8:(q + 1) * 8, :], aT[:], (aTB,), ())
            p.barrier()

        with ExitStack() as esB:
            p.cur_es = esB
            BW = Buf()
            w2 = esB.enter_context(nc.sbuf_tensor("w2_%d" % l, [128, 32, D], BF16))
            for k in range(32):
                p.dma("pool", w2[:, k, :], w2_d[l, k * 128:(k + 1) * 128, :], (), (BW,))
            xq_pool = TPool(p, "xr_%d" % l, [128, KD, TB], F32, 2)
            aT_pool = TPool(p, "aTr_%d" % l, [128, 32, TB], BF16, 2)
            for s in range(NSEQ):
                for blk in range(NBLK):
                    t0 = blk * TB
                    xb, xbB = xq_pool.next()
                    p.dma("sp", xb[:], fm(xs_d, s, t0, TB), (), (xbB,))
                    aT, aTB = aT_pool.next()
                    for q in range(4):
                        p.dma("sp", aT[:, q * 8:(q + 1) * 8, :], aT_d[s, blk, :, q * 8:(q + 1) * 8, :], (), (aTB,))
                    for m in range(KD):
                        po, poB = ps_pool.next()
                        for k in range(32):
                            mm(po[:], w2[:, k, m * 128:(m + 1) * 128], aT[:, k, :], k == 0, k == 31,
                               (BW, aTB), (poB,))
                        stt(xb[:, m, :], po[:], modT[:, l, 40 + m, s:s + 1], xb[:, m, :], ALU.mult, ALU.add,
                            (poB, B_mod, xbB), (xbB,))
                    p.dma("sp", fm(xs_d, s, t0, TB), xb[:], (xbB,), ())
                    if dbg and l == 0 and s == 0 and blk == want_dbg_blk:
                        p.dma("sp", dbg_d["d_x2"].rearrange("(k p) n -> p k n", p=128), xb[:], (xbB,), ())
            p.barrier()

    with ExitStack() as es2:
        p.cur_es = es2
        xq_pool = TPool(p, "fx", [128, KD, 128], F32, 2)
        fo_pool = TPool(p, "fo", [128, D], F32, 2)
        F32p = TPool(p, "f32p", [128, 128], F32, 6)
        RSF = TPool(p, "frs", [128, 128], F32, 2)
        F16p = TPool(p, "f16p", [128, 128], BF16, 4)
        for s in range(NSEQ):
            for tq in range(NQT):
                xb, xbB = xq_pool.next()
                p.dma("sp", xb[:], xs_d[s, tq // 4, :, :, (tq % 4) * 128:(tq % 4 + 1) * 128], (), (xbB,))
                pss, pssB = ps_pool.next()
                for k in range(KD):
                    sq, sqB = F16p.next()
                    act(sq[:], xb[:, k, :], AF.Square, (xbB,), (sqB,))
                    mm(pss[:, 0:128], ones_b[:], sq[:], k == 0, k == KD - 1, (sqB, B_const), (pssB,))
                rstd, rstdB = RSF.next()
                act(rstd[:], pss[:, 0:128], AF.Sqrt, (pssB,), (rstdB,), scale=1.0 / D, bias=EPS)
                p.op("dve", lambda E: E.reciprocal(out=rstd[:], in_=rstd[:]), (rstdB,), (rstdB,))
                fo, foB = fo_pool.next()
                for half in range(2):
                    pt, pb = ps_pool.next()
                    for kk in range(4):
                        k = half * 4 + kk
                        tmp, tmpB = F32p.next()
                        stt(tmp[:], xb[:, k, :], fnw[:, k:k + 1], rstd[:], ALU.mult, ALU.mult,
                            (xbB, B_const, rstdB), (tmpB,))
                        tr(pt[:, kk * 128:(kk + 1) * 128], tmp[:], ident_f[:], (tmpB, B_const), (pb,))
                    cp("act" if half == 0 else "dve", fo[:, half * 512:(half + 1) * 512], pt[:], (pb,), (foB,))
                p.dma("sp", out_d[s, tq * 128:(tq + 1) * 128, :], fo[:], (foB,), ())
        p.barrier()
    es.close()
    return nc, p


def pack_inputs(inputs, DEPTH, NSEQ, ncores):
    f = lambda a: np.ascontiguousarray(np.asarray(a, dtype=np.float32))
    x = f(inputs["x"]); c = f(inputs["c"])
    S = x.shape[1]

    def fmv(v):
        v = f(v)
        return v.reshape(-1, 128).T

    vecs = np.zeros((DEPTH, 128, NV), np.float32)
    for l in range(DEPTH):
        v = vecs[l]
        v[:, 0:8] = fmv(inputs["norm1_w"][l])
        v[:, 8:16] = fmv(inputs["norm2_w"][l])
        v[:, 16:24] = fmv(inputs["group_norm_w"][l])
        cw = f(inputs["conv_w"][l])
        for t in range(2):
            for k in range(4):
                v[:, 24 + t * 4 + k] = cw[k, t * 128:(t + 1) * 128]
        v[:, 32:34] = fmv(inputs["conv_b"][l])
        v[:, 34:36] = fmv(f(inputs["lru_ba"][l]).reshape(-1))
        v[:, 36:38] = fmv(f(inputs["lru_bx"][l]).reshape(-1))
        v[:, 38:40] = fmv(inputs["lru_lambda"][l])
        v[:, 40:42] = fmv(inputs["q_lat_norm_w"][l])
        v[:, 42:43] = fmv(inputs["kv_lat_norm_w"][l])
        v[:, 43:91] = fmv(inputs["b_ada"][l])
        v[0:4, 91] = f(inputs["mlstm_bi"][l])
        v[0:4, 92] = f(inputs["mlstm_bf"][l])
    shared = {
        "w_in": f(inputs["w_in"])[:DEPTH], "w_o": f(inputs["w_o"])[:DEPTH], "w_ada": f(inputs["w_ada"])[:DEPTH],
        "w_mlp1": f(inputs["w_mlp1"])[:DEPTH], "w_mlp2": f(inputs["w_mlp2"])[:DEPTH],
        "lru_wa": f(inputs["lru_wa"])[:DEPTH], "lru_wx": f(inputs["lru_wx"])[:DEPTH],
        "w_q_up": f(inputs["w_q_up"])[:DEPTH].reshape(DEPTH, 256, 256),
        "w_qidx_up": f(inputs["w_qidx_up"])[:DEPTH].reshape(DEPTH, 256, 256),
        "w_uk": f(inputs["w_uk"])[:DEPTH].reshape(DEPTH, 128, 256),
        "w_uv": f(inputs["w_uv"])[:DEPTH].reshape(DEPTH, 128, 256),
        "vec": vecs,
        "gmrow": np.ascontiguousarray(f(inputs["group_norm_w"])[:DEPTH, 256:768]),
        "kvrow": f(inputs["kv_lat_norm_w"])[:DEPTH],
        "relb": f(inputs["rel_bias"]).reshape(128),
        "fnw": np.ascontiguousarray(fmv(inputs["final_norm_w"])),
    }
    for k, v in make_consts().items():
        shared["c_" + k] = v
    in_maps = []
    for ci in range(ncores):
        d = dict(shared)
        d["x"] = np.ascontiguousarray(x[ci * NSEQ:(ci + 1) * NSEQ])
        cs = c[ci * NSEQ:(ci + 1) * NSEQ]
        d["cT"] = np.ascontiguousarray(cs.T.reshape(KD, 128, NSEQ).transpose(1, 0, 2))
        in_maps.append(d)
    return in_maps


_CACHE = {}


def kernel(**inputs):
    x = np.asarray(inputs["x"])
    B, S, _ = x.shape
    ncores = 8
    NSEQ = B // ncores
    key = (S, DEPTH_FULL, NSEQ)
    if key not in _CACHE:
        _CACHE[key] = build(S, DEPTH_FULL, NSEQ)[0]
    nc = _CACHE[key]
    in_maps = pack_inputs(inputs, DEPTH_FULL, NSEQ, ncores)
    res = run_bass_kernel_spmd(nc, in_maps, core_ids=list(range(ncores)))
    out = np.concatenate([np.asarray(r["out"]) for r in res.results], axis=0)
    return out.astype(np.float32)
```

```python
import math
from contextlib import ExitStack

import numpy as np
import concourse.bass as bass
import concourse.mybir as mybir
from concourse.bass_utils import run_bass_kernel_spmd

F32 = mybir.dt.float32
BF16 = mybir.dt.bfloat16
AF = mybir.ActivationFunctionType
ALU = mybir.AluOpType

D = 1024
KD = 8
DEPTH_FULL = 4
N_IN = 2992
O_LX, O_LY, O_Q, O_K, O_V, O_O, O_I, O_F, O_AQ, O_AKV, O_IK, O_IW = (
    0, 256, 512, 1024, 1536, 2048, 2560, 2564, 2568, 2824, 2952, 2984)
NV = 96
TB = 512
NIT = 16
EPS = 1e-6
NEG = -30000.0


class Buf:
    __slots__ = ("w", "r")

    def __init__(self):
        self.w = None
        self.r = {}


class Prog:
    EPOCH = 30000

    def __init__(self, nc, es):
        self.nc = nc
        self.es = es
        self.cur_es = es
        self.engs = {"pe": nc.tensor, "act": nc.scalar, "dve": nc.vector,
                     "pool": nc.gpsimd, "sp": nc.sync}
        self.sems = []
        self.cur = {}
        self.known = {e: {} for e in self.engs}
        for e in ("pe", "act", "dve", "pool"):
            self.cur[e] = [self._newsem(e), 0]
        self.slots = {"sp": [[self._newsem("dsp%d" % i), 0] for i in range(12)],
                      "pool": [[self._newsem("dpl%d" % i), 0] for i in range(8)]}
        self.slot_i = {"sp": 0, "pool": 0}
        self.n_ins = 0

    def _newsem(self, name):
        s = self.es.enter_context(self.nc.semaphore("s%d_%s" % (len(self.sems), name)))
        self.sems.append(s)
        return len(self.sems) - 1

    def _collect(self, eng, reads, writes, deps):
        known = self.known[eng]

        def need(ev):
            if ev is None:
                return
            sid, val, peng = ev
            if peng == eng and eng == "pe":
                return
            if known.get(sid, 0) >= val:
                return
            if deps.get(sid, 0) < val:
                deps[sid] = val

        for b in reads:
            need(b.w)
        for b in writes:
            need(b.w)
            for ev in b.r.values():
                need(ev)

    def _emit_waits(self, eng, deps):
        E = self.engs[eng]
        for sid, val in deps.items():
            E.wait_ge(self.sems[sid], val)
            self.known[eng][sid] = val
            self.n_ins += 1

    def _record(self, ev, key, reads, writes):
        for b in reads:
            b.r[key] = ev
        for b in writes:
            b.w = ev
            b.r = {}

    def op(self, eng, fn, reads=(), writes=()):
        deps = {}
        self._collect(eng, reads, writes, deps)
        self._emit_waits(eng, deps)
        ins = fn(self.engs[eng])
        c = self.cur[eng]
        c[1] += 1
        ins.then_inc(self.sems[c[0]], 1)
        ev = (c[0], c[1], eng)
        self._record(ev, eng, reads, writes)
        if c[1] >= self.EPOCH:
            self.cur[eng] = [self._newsem(eng), 0]
        self.n_ins += 1
        return ins

    def dma(self, q, out, in_, reads=(), writes=(), **kw):
        sl = self.slots[q][self.slot_i[q]]
        self.slot_i[q] = (self.slot_i[q] + 1) % len(self.slots[q])
        deps = {}
        if self.known[q].get(sl[0], 0) < sl[1]:
            deps[sl[0]] = sl[1]
        self._collect(q, reads, writes, deps)
        self._emit_waits(q, deps)
        ins = self.engs[q].dma_start(out=out, in_=in_, **kw)
        sl[1] += 16
        ins.then_inc(self.sems[sl[0]], 16)
        ev = (sl[0], sl[1], "dma")
        self._record(ev, ("dma", sl[0]), reads, writes)
        self.n_ins += 1
        return ins

    def barrier(self):
        evs = []
        for e, c in self.cur.items():
            if c[1] > 0:
                evs.append((c[0], c[1]))
        for q in self.slots:
            for sl in self.slots[q]:
                if sl[1] > 0:
                    evs.append((sl[0], sl[1]))
        for eng in self.engs:
            deps = {}
            for sid, val in evs:
                if self.known[eng].get(sid, 0) < val:
                    deps[sid] = val
            self._emit_waits(eng, deps)

    def final_wait(self, eng="sp"):
        self.barrier()


class TPool:
    def __init__(self, p, name, shape, dtype, n, psum=False):
        self.items = []
        for i in range(n):
            if psum:
                t = p.cur_es.enter_context(p.nc.psum_tensor("%s%d" % (name, i), shape, dtype))
            else:
                t = p.cur_es.enter_context(p.nc.sbuf_tensor("%s%d" % (name, i), shape, dtype))
            self.items.append((t, Buf()))
        self.i = 0

    def next(self):
        it = self.items[self.i]
        self.i = (self.i + 1) % len(self.items)
        return it


def t5_bucket_np(dist):
    n = np.maximum(dist, 0)
    log_ratio = np.log(np.maximum(n, 1).astype(np.float32) / np.float32(16)) / np.float32(math.log(128 / 16))
    large = 16 + (log_ratio * np.float32(16)).astype(np.int32)
    large = np.minimum(large, 31)
    return np.where(n < 16, n, large)


def make_consts():
    c = {}
    i = np.arange(128)
    c["ident"] = np.eye(128, dtype=np.float32)
    c["triu"] = (i[:, None] <= i[None, :]).astype(np.float32)
    c["negts"] = np.where(i[None, :] > i[:, None], np.float32(-1e30), np.float32(0)).astype(np.float32)
    d0 = i[None, :] - i[:, None]
    b0 = t5_bucket_np(d0).astype(np.float32)
    b0 = np.where(d0 >= 0, b0, np.float32(-1.0))
    c["bk0"] = b0.astype(np.float32)
    c["bk1"] = t5_bucket_np(d0 + 128).astype(np.float32)
    sel = np.zeros((8, 3, 128), np.float32)
    for h in range(8):
        sel[h, h // 3, (h % 3) * 32:(h % 3) * 32 + 32] = 1.0
    c["sel8"] = sel
    selh = np.zeros((4, 4, 128), np.float32)
    for h in range(4):
        selh[h, h, :] = 1.0
    c["selh"] = selh
    return c


CONST_SHAPES = {"ident": [128, 128], "triu": [128, 128], "negts": [128, 128], "bk0": [128, 128],
                "bk1": [128, 128], "sel8": [8, 3, 128], "selh": [4, 4, 128]}


def build(S, DEPTH, NSEQ, dbg=False):
    NBLK = S // TB
    NQT = S // 128
    TOPK = min(256, S // 4)
    nc = bass.Bass("TRN2", target_bir_lowering=False)
    es = ExitStack()
    p = Prog(nc, es)

    def din(name, shape):
        return nc.dram_tensor(name, shape, F32, kind="ExternalInput").ap()

    x_d = din("x", [NSEQ, S, D])
    cT_d = din("cT", [128, KD, NSEQ])
    w_in_d = din("w_in", [DEPTH, D, N_IN])
    w_o_d = din("w_o", [DEPTH, D, D])
    w_ada_d = din("w_ada", [DEPTH, D, 6 * D])
    w1_d = din("w_mlp1", [DEPTH, D, 4 * D])
    w2_d = din("w_mlp2", [DEPTH, 4 * D, D])
    wa_d = din("lru_wa", [DEPTH, 4, 64, 64])
    wx_d = din("lru_wx", [DEPTH, 4, 64, 64])
    wqup_d = din("w_q_up", [DEPTH, 256, 256])
    wqidx_d = din("w_qidx_up", [DEPTH, 256, 256])
    wuk_d = din("w_uk", [DEPTH, 128, 256])
    wuv_d = din("w_uv", [DEPTH, 128, 256])
    vec_d = din("vec", [DEPTH, 128, NV])
    gmrow_d = din("gmrow", [DEPTH, 512])
    kvrow_d = din("kvrow", [DEPTH, 128])
    relb_d = din("relb", [128])
    fnw_d = din("fnw", [128, KD])
    cd = {k: din("c_" + k, v) for k, v in CONST_SHAPES.items()}
    out_d = nc.dram_tensor("out", [NSEQ, S, D], F32, kind="ExternalOutput").ap()
    xs_d = nc.dram_tensor("xs", [NSEQ, NBLK, 128, KD, TB], F32).ap()
    dbg_d = {}
    if dbg:
        for nm, shp in (("d_proj", [N_IN, TB]), ("d_ylru", [256, TB]), ("d_ym", [512, TB]),
                        ("d_ya", [256, TB]), ("d_x1", [D, TB]), ("d_x2", [D, TB]),
                        ("d_sc", [128, S]), ("d_thr", [128, 4]), ("d_xb", [D, TB]), ("d_hT", [D, TB]),
                        ("d_mod", [128, 48]), ("d_qaT", [128, 4 * TB]), ("d_ckvT", [128, S]), ("d_olat", [128, 512]),
                        ("d_yasb", [256, TB]), ("d_pT", [128, 512]), ("d_mrow", [1, 4 * TB]), ("d_num", [128, 512]),
                        ("d_den", [128, 512])):
            dbg_d[nm] = nc.dram_tensor(nm, shp, F32, kind="ExternalOutput").ap()

    def sb(name, shape, dt=F32):
        return es.enter_context(nc.sbuf_tensor(name, shape, dt))

    def mm(out, lhsT, rhs, start, stop, reads, writes):
        p.op("pe", lambda E: E.matmul(out, lhsT=lhsT, rhs=rhs, start=start, stop=stop), reads, writes)

    def tr(out, in_, ident, reads, writes):
        p.op("pe", lambda E: E.transpose(out, in_, ident), reads, writes)

    def act(out, in_, func, reads, writes, **kw):
        p.op("act", lambda E: E.activation(out=out, in_=in_, func=func, **kw), reads, writes)

    def ts(out, in0, s1, s2, op0, op1, reads, writes, eng="dve", **kw):
        if op1 is None:
            p.op(eng, lambda E: E.tensor_scalar(out=out, in0=in0, scalar1=s1, scalar2=None, op0=op0, **kw),
                 reads, writes)
        else:
            p.op(eng, lambda E: E.tensor_scalar(out=out, in0=in0, scalar1=s1, scalar2=s2, op0=op0, op1=op1, **kw),
                 reads, writes)

    def tt(out, in0, in1, op, reads, writes, eng="dve"):
        p.op(eng, lambda E: E.tensor_tensor(out=out, in0=in0, in1=in1, op=op), reads, writes)

    def stt(out, in0, scalar, in1, op0, op1, reads, writes):
        p.op("dve", lambda E: E.scalar_tensor_tensor(out=out, in0=in0, scalar=scalar, in1=in1, op0=op0, op1=op1),
             reads, writes)

    def cp(eng, out, in_, reads, writes):
        if eng == "act":
            p.op("act", lambda E: E.copy(out=out, in_=in_), reads, writes)
        else:
            p.op(eng, lambda E: E.tensor_copy(out=out, in_=in_), reads, writes)

    def mset(eng, ap, val, writes):
        p.op(eng, lambda E: E.memset(ap, val), (), writes)

    ident_f = sb("ident_f", [128, 128]); B_const = Buf()
    ident_b = sb("ident_b", [128, 128], BF16)
    triu_b = sb("triu_b", [128, 128], BF16)
    negts = sb("negts", [128, 128])
    sel8 = sb("sel8", [8, 3, 128])
    selh = sb("selh", [4, 4, 128])
    ones_b = sb("ones_b", [128, 128], BF16)
    ones_f = sb("ones_f", [128, 512])
    identx4 = sb("identx4", [128, 4, 128], BF16)
    BT0 = sb("BT0", [128, 4, 128], BF16)
    BT1 = sb("BT1", [128, 4, 128], BF16)
    relb_bc = sb("relb_bc", [128, 128])
    relb_row = sb("relb_row", [1, 128])
    bmax = sb("bmax", [1, 1])
    fnw = sb("fnw_sb", [128, KD])
    modT = sb("modT", [128, DEPTH, 48, NSEQ])
    B_mod = Buf()

    ps_pool = TPool(p, "ps", [128, 512], F32, 6, psum=True)
    acc_pool = TPool(p, "psacc", [128, 512], F32, 2, psum=True)

    p.dma("sp", ident_f[:], cd["ident"][:, :], (), (B_const,))
    p.dma("sp", negts[:], cd["negts"][:, :], (), (B_const,))
    p.dma("sp", sel8[:], cd["sel8"][:, :, :], (), (B_const,))
    p.dma("sp", selh[:], cd["selh"][:, :, :], (), (B_const,))
    p.dma("sp", fnw[:], fnw_d[:, :], (), (B_const,))
    p.dma("sp", relb_bc[:], relb_d.partition_broadcast(128), (), (B_const,))
    p.dma("sp", relb_row[:], relb_d.rearrange("(o n) -> o n", o=1), (), (B_const,))
    p.dma("pool", ident_b[:], cd["ident"][:, :], (), (B_const,))
    p.dma("pool", triu_b[:], cd["triu"][:, :], (), (B_const,))
    mset("dve", ones_b[:], 1.0, (B_const,))
    mset("dve", ones_f[:], 1.0, (B_const,))
    for h in range(4):
        ts(identx4[:, h, :], ident_f[:], 30000.0, None, ALU.mult, None, (B_const,), (B_const,))
    p.op("dve", lambda E: E.tensor_reduce(out=bmax[:], in_=relb_row[:], axis=mybir.AxisListType.X, op=ALU.max,
                                          apply_absolute_value=True), (B_const,), (B_const,))
    with ExitStack() as es2:
        p.cur_es = es2
        bk0 = es2.enter_context(nc.sbuf_tensor("bk0", [128, 128], F32))
        bk1 = es2.enter_context(nc.sbuf_tensor("bk1", [128, 128], F32))
        acc = es2.enter_context(nc.sbuf_tensor("bacc", [128, 2, 4, 128], F32))
        tmpb = es2.enter_context(nc.sbuf_tensor("btmp", [128, 128], F32))
        p.dma("sp", bk0[:], cd["bk0"][:, :], (), (B_const,))
        p.dma("sp", bk1[:], cd["bk1"][:, :], (), (B_const,))
        for h in range(4):
            ts(acc[:, 0, h, :], bk0[:], -1.0, NEG, ALU.is_equal, ALU.mult, (B_const,), (B_const,))
            mset("dve", acc[:, 1, h, :], 0.0, (B_const,))
        for which, bk in ((0, bk0), (1, bk1)):
            for b in range(32):
                for h in range(4):
                    ts(tmpb[:], bk[:], float(b), relb_bc[:, b * 4 + h:b * 4 + h + 1], ALU.is_equal, ALU.mult,
                       (B_const,), (B_const,))
                    tt(acc[:, which, h, :], acc[:, which, h, :], tmpb[:], ALU.add, (B_const,), (B_const,))
        for which in range(2):
            for h in range(4):
                ts(acc[:, which, h, :], acc[:, which, h, :], relb_bc[:, 124 + h:125 + h], None, ALU.subtract, None,
                   (B_const,), (B_const,))
        for h in range(4):
            cp("dve", BT0[:, h, :], acc[:, 0, h, :], (B_const,), (B_const,))
            cp("dve", BT1[:, h, :], acc[:, 1, h, :], (B_const,), (B_const,))
        p.barrier()

    with ExitStack() as es2:
        p.cur_es = es2
        cT = es2.enter_context(nc.sbuf_tensor("cT_sb", [128, KD, NSEQ], F32))
        cTb = es2.enter_context(nc.sbuf_tensor("cTb", [128, KD, NSEQ], F32))
        wad = [es2.enter_context(nc.sbuf_tensor("wad%d" % i, [128, KD, 768], F32)) for i in range(3)]
        wadB = [Buf(), Buf(), Buf()]
        vecs = es2.enter_context(nc.sbuf_tensor("vecs0", [128, DEPTH, NV], F32))
        B_c = Buf()
        p.dma("sp", cT[:], cT_d[:, :, :], (), (B_c,))
        for l in range(DEPTH):
            p.dma("sp", vecs[:, l, :], vec_d[l, :, :], (), (B_c,))
        act(cTb[:], cT[:], AF.Silu, (B_c,), (B_c,))
        it = 0
        for l in range(DEPTH):
            for cc in range(8):
                wt, wb = wad[it % 3], wadB[it % 3]
                it += 1
                for k in range(KD):
                    p.dma("sp", wt[:, k, :], w_ada_d[l, k * 128:(k + 1) * 128, cc * 768:(cc + 1) * 768], (), (wb,))
                for jj in range(6):
                    j = cc * 6 + jj
                    pt, pb = ps_pool.next()
                    for k in range(KD):
                        mm(pt[:, 0:NSEQ], wt[:, k, jj * 128:(jj + 1) * 128], cTb[:, k, :], k == 0, k == KD - 1,
                           (wb, B_c), (pb,))
                    ts(modT[:, l, j, :], pt[:, 0:NSEQ], vecs[:, l, 43 + j:44 + j], None, ALU.add, None,
                       (pb, B_c), (B_mod,))
        for l in range(DEPTH):
            for base in (8, 32):
                ts(modT[:, l, base:base + 8, :], modT[:, l, base:base + 8, :], 1.0, None, ALU.add, None,
                   (B_mod,), (B_mod,))
        p.barrier()

    with ExitStack() as es2:
        p.cur_es = es2
        xin = [es2.enter_context(nc.sbuf_tensor("xin%d" % i, [128, D], F32)) for i in range(3)]
        xinB = [Buf() for _ in range(3)]
        xo = [es2.enter_context(nc.sbuf_tensor("xo%d" % i, [128, KD, 128], F32)) for i in range(3)]
        xoB = [Buf() for _ in range(3)]
        it = 0
        for s in range(NSEQ):
            for tq in range(NQT):
                a, ab = xin[it % 3], xinB[it % 3]
                o, ob = xo[it % 3], xoB[it % 3]
                it += 1
                p.dma("sp", a[:], x_d[s, tq * 128:(tq + 1) * 128, :], (), (ab,))
                for half in range(2):
                    pt, pb = ps_pool.next()
                    for kk in range(4):
                        k = half * 4 + kk
                        tr(pt[:, kk * 128:(kk + 1) * 128], a[:, k * 128:(k + 1) * 128], ident_f[:], (ab, B_const), (pb,))
                    cp("act" if half == 0 else "dve", o[:, half * 4:half * 4 + 4, :],
                       pt[:].rearrange("p (k n) -> p k n", k=4), (pb,), (ob,))
                p.dma("sp", xs_d[s, tq // 4, :, :, (tq % 4) * 128:(tq % 4 + 1) * 128], o[:],
                      (ob,), ())
        p.barrier()

    hT_d = nc.dram_tensor("hT_s", [NSEQ, NBLK, 128, KD, TB], BF16).ap()
    yT_d = nc.dram_tensor("yT_s", [NSEQ, NBLK, 128, KD, TB], BF16).ap()
    want_dbg_blk = 1 if NBLK > 1 else 0

    def fm(ap_d, s, c0, n):
        return ap_d[s, c0 // TB, :, :, (c0 % TB):(c0 % TB) + n]

    def rms_bcast(T16, T32, tiles, n_feat, width=TB):
        pss, pssB = ps_pool.next()
        for i, (a, aB) in enumerate(tiles):
            sq, sqB = T16.next()
            act(sq[:, 0:width], a, AF.Square, (aB,), (sqB,))
            mm(pss[:, 0:width], ones_b[:], sq[:, 0:width], i == 0, i == len(tiles) - 1, (sqB, B_const), (pssB,))
        rs, rsB = T32.next()
        act(rs[:, 0:width], pss[:, 0:width], AF.Sqrt, (pssB,), (rsB,), scale=1.0 / n_feat, bias=EPS)
        p.op("dve", lambda E: E.reciprocal(out=rs[:, 0:width], in_=rs[:, 0:width]), (rsB,), (rsB,))
        return rs, rsB

    for l in range(DEPTH):
        with ExitStack() as esA:
            p.cur_es = esA
            vec = esA.enter_context(nc.sbuf_tensor("vec0_%d" % l, [128, NV], F32)); BW = Buf()
            p.dma("sp", vec[:], vec_d[l, :, :], (), (BW,))
            T32 = TPool(p, "p0t32_%d" % l, [128, 512], F32, 4)
            RS0 = TPool(p, "p0rs_%d" % l, [128, 512], F32, 2)
            T16 = TPool(p, "p0t16_%d" % l, [128, 512], BF16, 4)
            xblk_pool = TPool(p, "p0x_%d" % l, [128, KD, TB], F32, 2)
            hT_pool = TPool(p, "p0h_%d" % l, [128, KD, TB], BF16, 2)
            for s in range(NSEQ):
                A1w = esA.enter_context(nc.sbuf_tensor("A1w_%d_%d" % (l, s), [128, 8], F32)); A1wB = Buf()
                tt(A1w[:, 0:8], modT[:, l, 8:16, s], vec[:, 0:8], ALU.mult, (B_mod, BW), (A1wB,))
                for blk in range(NBLK):
                    t0 = blk * TB
                    xb, xbB = xblk_pool.next()
                    p.dma("sp", xb[:], fm(xs_d, s, t0, TB), (), (xbB,))
                    hT, hTB = hT_pool.next()
                    rstd, rstdB = rms_bcast(T16, RS0, [(xb[:, k, :], xbB) for k in range(KD)], D)
                    for k in range(KD):
                        tmp, tmpB = T32.next()
                        tt(tmp[:], xb[:, k, :], rstd[:], ALU.mult, (xbB, rstdB), (tmpB,))
                        act(hT[:, k, :], tmp[:], AF.Identity, (tmpB, A1wB, B_mod), (hTB,),
                            scale=A1w[:, k:k + 1], bias=modT[:, l, k, s:s + 1])
                    p.dma("sp", fm(hT_d, s, t0, TB), hT[:], (hTB,), ())
                    if dbg and l == 0 and s == 0 and blk == want_dbg_blk:
                        p.dma("sp", dbg_d["d_xb"].rearrange("(k p) n -> p k n", p=128), xb[:], (xbB,), ())
                        p.dma("pool", dbg_d["d_hT"].rearrange("(k p) n -> p k n", p=128), hT[:], (hTB,), ())
                        p.dma("sp", dbg_d["d_mod"][:, :], modT[:, 0, :, 0], (B_mod,), ())
            p.barrier()

        def load_w_in(esX, c0, ncol, BW, name):
            w = esX.enter_context(nc.sbuf_tensor("%s_%d" % (name, l), [128, KD, ncol], BF16))
            for k in range(KD):
                p.dma("pool", w[:, k, :], w_in_d[l, k * 128:(k + 1) * 128, c0:c0 + ncol], (), (BW,))
            return w

        def dbg_on(s, blk):
            return dbg and l == 0 and s == 0 and blk == want_dbg_blk

        with ExitStack() as esA:
            p.cur_es = esA
            def sa(name, shape, dt=F32):
                return esA.enter_context(nc.sbuf_tensor("%s_%d" % (name, l), shape, dt))
            BW = Buf()
            wl = load_w_in(esA, 0, 512, BW, "wlru")
            waBD = sa("waBD", [128, 2, 128], BF16)
            wxBD = sa("wxBD", [128, 2, 128], BF16)
            vec = sa("vec1", [128, NV], F32)
            lcl = sa("lcl", [128, 2], F32)
            ltmp = sa("ltmp", [128, 8], F32)
            p.dma("sp", vec[:], vec_d[l, :, :], (), (BW,))
            mset("dve", waBD[:], 0.0, (BW,))
            mset("dve", wxBD[:], 0.0, (BW,))
            for n in range(4):
                r0 = (n % 2) * 64
                p.dma("pool", waBD[r0:r0 + 64, n // 2, r0:r0 + 64], wa_d[l, n, :, :], (), (BW,))
                p.dma("pool", wxBD[r0:r0 + 64, n // 2, r0:r0 + 64], wx_d[l, n, :, :], (), (BW,))
            act(ltmp[:, 0:2], vec[:, 38:40], AF.Exp, (BW,), (BW,), scale=-1.0)
            act(ltmp[:, 2:4], ltmp[:, 0:2], AF.Ln, (BW,), (BW,), bias=1.0)
            ts(ltmp[:, 4:6], ltmp[:, 0:2], -0.25, 1.0 / 3.0, ALU.mult, ALU.add, (BW,), (BW,))
            tt(ltmp[:, 4:6], ltmp[:, 4:6], ltmp[:, 0:2], ALU.mult, (BW,), (BW,))
            ts(ltmp[:, 4:6], ltmp[:, 4:6], -1.0, 0.5, ALU.mult, ALU.add, (BW,), (BW,))
            tt(ltmp[:, 4:6], ltmp[:, 4:6], ltmp[:, 0:2], ALU.mult, (BW,), (BW,))
            ts(ltmp[:, 4:6], ltmp[:, 4:6], -1.0, 1.0, ALU.mult, ALU.add, (BW,), (BW,))
            tt(ltmp[:, 4:6], ltmp[:, 4:6], ltmp[:, 0:2], ALU.mult, (BW,), (BW,))
            ts(ltmp[:, 6:8], ltmp[:, 0:2], 0.1, None, ALU.is_lt, None, (BW,), (BW,))
            tt(ltmp[:, 4:6], ltmp[:, 4:6], ltmp[:, 2:4], ALU.subtract, (BW,), (BW,))
            tt(ltmp[:, 4:6], ltmp[:, 4:6], ltmp[:, 6:8], ALU.mult, (BW,), (BW,))
            tt(ltmp[:, 4:6], ltmp[:, 4:6], ltmp[:, 2:4], ALU.add, (BW,), (BW,))
            ts(lcl[:], ltmp[:, 4:6], -8.0, None, ALU.mult, None, (BW,), (BW,))

            T32 = TPool(p, "p1t32_%d" % l, [128, 512], F32, 24)
            T16 = TPool(p, "p1t16_%d" % l, [128, 512], BF16, 4)
            hT_pool = TPool(p, "p1h_%d" % l, [128, KD, TB], BF16, 2)
            yo_pool = TPool(p, "p1y_%d" % l, [128, 2, TB], BF16, 2)
            lxbuf = sa("lxbuf", [128, 2, 3 + TB], F32); B_lxt = [Buf(), Buf()]
            lru_h = sa("lru_h", [128, 2], F32); B_lht = [Buf(), Buf()]
            for s in range(NSEQ):
                mset("dve", lxbuf[:, :, 0:3], 0.0, tuple(B_lxt))
                mset("dve", lru_h[:], 0.0, tuple(B_lht))
                for blk in range(NBLK):
                    t0 = blk * TB
                    hT, hTB = hT_pool.next()
                    p.dma("sp", hT[:], fm(hT_d, s, t0, TB), (), (hTB,))
                    yo, yoB = yo_pool.next()

                    def proj(wt, c0, ncol):
                        pt, pb = ps_pool.next()
                        for k in range(KD):
                            mm(pt[0:ncol, :], wt[:, k, c0:c0 + ncol], hT[:, k, :], k == 0, k == KD - 1, (BW, hTB), (pb,))
                        return pt, pb

                    ylru = []
                    TT = range(2)
                    xc = [T32.next() for t in TT]
                    rr = [T32.next() for t in TT]
                    gi = [T32.next() for t in TT]
                    aa = [T32.next() for t in TT]
                    a2 = [T32.next() for t in TT]
                    hnew = [T32.next() for t in TT]
                    yv = [T32.next() for t in TT]
                    y2 = [T32.next() for t in TT]
                    xcb = [T16.next() for t in TT]
                    for t in TT:
                        pt, pb = proj(wl, t * 128, 128)
                        cp("act", lxbuf[:, t, 3:3 + TB], pt[:], (pb,), (B_lxt[t],))
                        if dbg_on(s, blk):
                            p.dma("sp", dbg_d["d_proj"][t * 128:(t + 1) * 128, :], lxbuf[:, t, 3:3 + TB], (B_lxt[t],), ())
                    for t in TT:
                        ts(xc[t][0][:], lxbuf[:, t, 0:TB], vec[:, 24 + t * 4:25 + t * 4], vec[:, 32 + t:33 + t],
                           ALU.mult, ALU.add, (B_lxt[t], BW), (xc[t][1],))
                        for kk in range(1, 4):
                            stt(xc[t][0][:], lxbuf[:, t, kk:kk + TB], vec[:, 24 + t * 4 + kk:25 + t * 4 + kk], xc[t][0][:],
                                ALU.mult, ALU.add, (B_lxt[t], BW, xc[t][1]), (xc[t][1],))
                        cp("act", lxbuf[:, t, 0:3], lxbuf[:, t, TB:TB + 3], (B_lxt[t],), (B_lxt[t],))
                        cp("act", xcb[t][0][:], xc[t][0][:], (xc[t][1],), (xcb[t][1],))
                    pgs = []
                    for t in TT:
                        pr, prB = ps_pool.next()
                        mm(pr[:], waBD[:, t, :], xcb[t][0][:], True, True, (BW, xcb[t][1]), (prB,))
                        pg, pgB = ps_pool.next()
                        mm(pg[:], wxBD[:, t, :], xcb[t][0][:], True, True, (BW, xcb[t][1]), (pgB,))
                        pgs.append((pr, prB, pg, pgB))
                    for t in TT:
                        pr, prB, pg, pgB = pgs[t]
                        act(rr[t][0][:], pr[:], AF.Sigmoid, (prB, BW), (rr[t][1],), bias=vec[:, 34 + t:35 + t])
                        act(gi[t][0][:], pg[:], AF.Sigmoid, (pgB, BW), (gi[t][1],), bias=vec[:, 36 + t:37 + t])
                    for t in TT:
                        act(aa[t][0][:], rr[t][0][:], AF.Exp, (rr[t][1], BW), (aa[t][1],), scale=lcl[:, t:t + 1])
                    for t in TT:
                        act(a2[t][0][:], aa[t][0][:], AF.Square, (aa[t][1],), (a2[t][1],))
                    for t in TT:
                        act(a2[t][0][:], a2[t][0][:], AF.Sqrt, (a2[t][1],), (a2[t][1],), scale=-1.0, bias=1.0)
                    pys = [proj(wl, 256 + t * 128, 128) for t in TT]
                    for t in TT:
                        tt(gi[t][0][:], gi[t][0][:], xc[t][0][:], ALU.mult, (gi[t][1], xc[t][1]), (gi[t][1],))
                        tt(gi[t][0][:], gi[t][0][:], a2[t][0][:], ALU.mult, (gi[t][1], a2[t][1]), (gi[t][1],))
                        p.op("dve", lambda E, t=t: E.tensor_tensor_scan(out=hnew[t][0][:], data0=aa[t][0][:], data1=gi[t][0][:],
                                                                        initial=lru_h[:, t:t + 1],
                                                                        op0=ALU.mult, op1=ALU.add),
                             (aa[t][1], gi[t][1], B_lht[t]), (hnew[t][1],))
                        cp("act", lru_h[:, t:t + 1], hnew[t][0][:, TB - 1:TB], (hnew[t][1],), (B_lht[t],))
                    for t in TT:
                        cp("act", yv[t][0][:], pys[t][0][:], (pys[t][1],), (yv[t][1],))
                        act(y2[t][0][:], yv[t][0][:], AF.Square, (yv[t][1],), (y2[t][1],))
                    for t in TT:
                        ts(y2[t][0][:], y2[t][0][:], 0.044715, 1.0, ALU.mult, ALU.add, (y2[t][1],), (y2[t][1],))
                        tt(y2[t][0][:], y2[t][0][:], yv[t][0][:], ALU.mult, (y2[t][1], yv[t][1]), (y2[t][1],))
                    for t in TT:
                        act(y2[t][0][:], y2[t][0][:], AF.Sigmoid, (y2[t][1],), (y2[t][1],), scale=1.5957691216057308)
                    for t in TT:
                        tt(y2[t][0][:], y2[t][0][:], yv[t][0][:], ALU.mult, (y2[t][1], yv[t][1]), (y2[t][1],))
                        tt(hnew[t][0][:], hnew[t][0][:], y2[t][0][:], ALU.mult, (hnew[t][1], y2[t][1]), (hnew[t][1],))
                        ylru.append(hnew[t])
                    rs, rsB = rms_bcast(T16, T32, [(ylru[t][0][:], ylru[t][1]) for t in range(2)], 256)
                    for t in range(2):
                        stt(yo[:, t, :], ylru[t][0][:], vec[:, 16 + t:17 + t], rs[:], ALU.mult, ALU.mult,
                            (ylru[t][1], BW, rsB), (yoB,))
                        if dbg_on(s, blk):
                            tmp, tmpB = T32.next()
                            cp("dve", tmp[:], yo[:, t, :], (yoB,), (tmpB,))
                            p.dma("sp", dbg_d["d_ylru"][t * 128:(t + 1) * 128, :], tmp[:], (tmpB,), ())
                    p.dma("sp", yT_d[s, blk, :, 0:2, :], yo[:], (yoB,), ())
            p.barrier()

        with ExitStack() as esA:
            p.cur_es = esA
            def sa(name, shape, dt=F32):
                return esA.enter_context(nc.sbuf_tensor("%s_%d" % (name, l), shape, dt))
            BW = Buf()
            wm = load_w_in(esA, O_Q, O_AQ - O_Q, BW, "wml")
            vec = sa("vec2", [128, NV], F32)
            gm_bc = sa("gm_bc", [128, 512], F32)
            gb15 = sa("gb15", [4, 2], F32)
            p.dma("sp", vec[:], vec_d[l, :, :], (), (BW,))
            p.dma("sp", gm_bc[:], gmrow_d[l, :].partition_broadcast(128), (), (BW,))
            ts(gb15[:], vec[0:4, 91:93], 1.0 / 15.0, None, ALU.mult, None, (BW,), (BW,))
            T32 = TPool(p, "p2t32_%d" % l, [128, 132], F32, 28)
            T16 = TPool(p, "p2t16_%d" % l, [128, 132], BF16, 36)
            hT_pool = TPool(p, "p2h_%d" % l, [128, KD, TB], BF16, 2)
            yo_pool = TPool(p, "p2y_%d" % l, [128, 4, TB], BF16, 2)
            qkT = sa("qkT", [128, 8, TB], BF16); B_qk = Buf()
            oT = sa("oT", [128, 4, TB], BF16); B_oT = Buf()
            vk_tok = sa("vk_tok", [128, 4, 2, 4, 130], BF16); B_vk = Buf()
            Cn = sa("Cn", [128, 4, 130], F32); B_Cnh = [Buf() for _ in range(4)]
            Cnb = sa("Cnb", [128, 4, 130], BF16)
            gates = sa("gates", [4, 4, TB], F32); B_g = Buf()
            tokg = sa("tokg", [128, 4, 12], F32); B_tg = Buf()
            for s in range(NSEQ):
                mset("dve", Cn[:], 0.0, tuple(B_Cnh))
                mset("dve", vk_tok[:, :, 0, :, 128:130], 1.0, (B_vk,))
                for blk in range(NBLK):
                    t0 = blk * TB
                    hT, hTB = hT_pool.next()
                    p.dma("sp", hT[:], fm(hT_d, s, t0, TB), (), (hTB,))
                    yo, yoB = yo_pool.next()

                    def proj(c0, ncol):
                        pt, pb = ps_pool.next()
                        for k in range(KD):
                            mm(pt[0:ncol, :], wm[:, k, c0:c0 + ncol], hT[:, k, :], k == 0, k == KD - 1, (BW, hTB), (pb,))
                        return pt, pb

                    for h in range(4):
                        pt, pb = proj(h * 128, 128)
                        act(qkT[:, h, :], pt[:], AF.Copy, (pb,), (B_qk,), scale=128.0 ** -0.5)
                        pt, pb = proj(512 + h * 128, 128)
                        cp("dve", qkT[:, 4 + h, :], pt[:], (pb,), (B_qk,))
                        pt, pb = proj(1536 + h * 128, 128)
                        act(oT[:, h, :], pt[:], AF.Sigmoid, (pb,), (B_oT,))
                    for c in range(4):
                        for which, col in ((0, 1024), (1, 512)):
                            pt, pb = ps_pool.next()
                            for k in range(KD):
                                mm(pt[:], hT[:, k, c * 128:(c + 1) * 128], wm[:, k, col:col + 512],
                                   k == 0, k == KD - 1, (hTB, BW), (pb,))
                            cp("act" if which == 0 else "dve", vk_tok[:, c, which, :, 0:128],
                               pt[:].rearrange("p (h d) -> p h d", h=4), (pb,), (B_vk,))
                    pi_, piB = proj(2048, 4)
                    act(gates[:, 0, :], pi_[0:4, :], AF.Tanh, (piB, BW), (B_g,), scale=1.0 / 15.0, bias=gb15[:, 0:1])
                    ts(gates[:, 0, :], gates[:, 0, :], 15.0, None, ALU.mult, None, (B_g,), (B_g,))
                    pf_, pfB = proj(2052, 4)
                    act(gates[:, 3, :], pf_[0:4, :], AF.Tanh, (pfB, BW), (B_g,), scale=1.0 / 15.0, bias=gb15[:, 1:2])
                    act(gates[:, 3, :], gates[:, 3, :], AF.Exp, (B_g,), (B_g,), scale=-15.0)
                    act(gates[:, 3, :], gates[:, 3, :], AF.Ln, (B_g,), (B_g,), bias=1.0)
                    ts(gates[:, 1, :], gates[:, 3, :], -1.0, None, ALU.mult, None, (B_g,), (B_g,))
                    for c in range(4):
                        p.op("dve", lambda E: E.tensor_tensor_scan(
                            out=gates[:, 2, c * 128:(c + 1) * 128], data0=ones_f[0:4, c * 128:(c + 1) * 128],
                            data1=gates[:, 1, c * 128:(c + 1) * 128], initial=0.0, op0=ALU.mult, op1=ALU.add),
                             (B_g, B_const), (B_g,))
                    tt(gates[:, 3, :], gates[:, 0, :], gates[:, 2, :], ALU.subtract, (B_g,), (B_g,))
                    for c in range(4):
                        pt, pb = ps_pool.next()
                        mm(pt[:, 0:4], gates[:, 3, c * 128:(c + 1) * 128], ident_f[0:4, 0:4], True, True,
                           (B_g, B_const), (pb,))
                        mm(pt[:, 4:8], gates[:, 2, c * 128:(c + 1) * 128], ident_f[0:4, 0:4], True, True,
                           (B_g, B_const), (pb,))
                        dgl, dglB = T32.next()
                        ts(dgl[0:4, 0:4], ident_f[0:4, 0:4], gates[:, 2, c * 128 + 127:c * 128 + 128], None,
                           ALU.mult, None, (B_g, B_const), (dglB,))
                        mm(pt[:, 8:12], ones_f[0:4, 0:128], dgl[0:4, 0:4], True, True, (dglB, B_const), (pb,))
                        act(tokg[:, c, :], pt[:, 0:12], AF.Exp, (pb,), (B_tg,))
                    for c in range(4):
                        cs = slice(c * 128, (c + 1) * 128)
                        bS, bSB = ps_pool.next()
                        bO = [ps_pool.next(), ps_pool.next()]
                        bU = [ps_pool.next(), ps_pool.next()]
                        bT, bTB = ps_pool.next()
                        bTb = bT[:].bitcast(BF16)
                        H = range(4)
                        vp = [T16.next() for h in H]
                        kb = [T16.next() for h in H]
                        P0 = [T16.next() for h in H]
                        hn = [T16.next() for h in H]
                        sm = [T32.next() for h in H]
                        ho = [T32.next() for h in H]
                        junk = [T32.next() for h in H]

                        def pOv(h):
                            return bO[h // 2][0][:, (h % 2) * 130:(h % 2) * 130 + 130], bO[h // 2][1]

                        def pUv(h):
                            return bU[h // 2][0][:, (h % 2) * 130:(h % 2) * 130 + 130], bU[h // 2][1]

                        for h in H:
                            ts(vp[h][0][:, 0:130], vk_tok[:, c, 0, h, :], tokg[:, c, h:h + 1], None, ALU.mult, None,
                               (B_vk, B_tg), (vp[h][1],))
                            cp("act", kb[h][0][:, 0:128], vk_tok[:, c, 1, h, 0:128], (B_vk,), (kb[h][1],))
                            cp("act", Cnb[:, h, :], Cn[:, h, :], (B_Cnh[h],), (B_Cnh[h],))
                            mm(bS[:, h * 128:(h + 1) * 128], qkT[:, 4 + h, cs], qkT[:, h, cs], True, True, (B_qk,), (bSB,))
                        for h in H:
                            tt(P0[h][0][:, 0:128], bS[:, h * 128:(h + 1) * 128], triu_b[:], ALU.mult,
                               (bSB, B_const), (P0[h][1],))
                        for h in H:
                            po, poB = pOv(h)
                            mm(po, P0[h][0][:, 0:128], vp[h][0][:, 0:130], True, False, (P0[h][1], vp[h][1]), (poB,))
                            mm(po, qkT[:, h, cs], Cnb[:, h, :], False, True, (B_qk, B_Cnh[h]), (poB,))
                            pu, puB = pUv(h)
                            mm(pu, kb[h][0][:, 0:128], vp[h][0][:, 0:130], True, True, (kb[h][1], vp[h][1]), (puB,))
                        for h in H:
                            pu, puB = pUv(h)
                            tt(Cn[:, h, :], Cn[:, h, :], pu, ALU.add, (B_Cnh[h], puB), (B_Cnh[h],))
                            ts(Cn[:, h, :], Cn[:, h, :], tokg[:, c, 8 + h:9 + h], None, ALU.mult, None,
                               (B_Cnh[h], B_tg), (B_Cnh[h],))
                        for h in H:
                            po, poB = pOv(h)
                            act(sm[h][0][:, 0:1], po[:, 128:129], AF.Abs, (poB, B_tg), (sm[h][1],),
                                scale=tokg[:, c, 4 + h:5 + h])
                        for h in H:
                            po, poB = pOv(h)
                            smt, smB = sm[h]
                            ts(smt[:, 0:1], smt[:, 0:1], 1.0, None, ALU.max, None, (smB,), (smB,))
                            p.op("dve", lambda E: E.reciprocal(out=smt[:, 1:2], in_=smt[:, 0:1]), (smB,), (smB,))
                            tt(smt[:, 2:3], smt[:, 1:2], tokg[:, c, 4 + h:5 + h], ALU.mult, (smB, B_tg), (smB,))
                            ts(ho[h][0][:, 0:128], po[:, 0:128], smt[:, 2:3], None, ALU.mult, None, (poB, smB), (ho[h][1],))
                        for h in H:
                            smt, smB = sm[h]
                            act(junk[h][0][:, 0:128], ho[h][0][:, 0:128], AF.Square, (ho[h][1],), (junk[h][1], smB),
                                accum_out=smt[:, 3:4])
                            act(smt[:, 4:5], smt[:, 3:4], AF.Sqrt, (smB,), (smB,), scale=1.0 / 128, bias=EPS)
                        for h in H:
                            smt, smB = sm[h]
                            p.op("dve", lambda E: E.reciprocal(out=smt[:, 5:6], in_=smt[:, 4:5]), (smB,), (smB,))
                            stt(hn[h][0][:, 0:128], ho[h][0][:, 0:128], smt[:, 5:6], gm_bc[:, h * 128:(h + 1) * 128],
                                ALU.mult, ALU.mult, (ho[h][1], smB, BW), (hn[h][1],))
                        for h in H:
                            tr(bTb[:, h * 128:(h + 1) * 128], hn[h][0][:, 0:128], ident_b[:], (hn[h][1], B_const), (bTB,))
                        for h in H:
                            tt(yo[:, h, cs], bTb[:, h * 128:(h + 1) * 128], oT[:, h, cs], ALU.mult, (bTB, B_oT), (yoB,))
                    if dbg_on(s, blk):
                        for h in range(4):
                            for half in range(4):
                                tmp, tmpB = T32.next()
                                cp("dve", tmp[:, 0:128], yo[:, h, half * 128:(half + 1) * 128], (yoB,), (tmpB,))
                                p.dma("sp", dbg_d["d_ym"][h * 128:(h + 1) * 128, half * 128:(half + 1) * 128],
                                      tmp[:, 0:128], (tmpB,), ())
                    p.dma("sp", yT_d[s, blk, :, 2:6, :], yo[:], (yoB,), ())
            p.barrier()

        with ExitStack() as esA:
            p.cur_es = esA
            def sa(name, shape, dt=F32):
                return esA.enter_context(nc.sbuf_tensor("%s_%d" % (name, l), shape, dt))
            BW = Buf()
            wd = load_w_in(esA, O_AQ, N_IN - O_AQ, BW, "wdsa")
            w_ik4 = sa("w_ik4", [128, KD, 128], BF16)
            wqup = sa("wqup", [128, 2, 256], BF16)
            wqidx = sa("wqidx", [128, 2, 256], BF16)
            wqidx3 = sa("wqidx3", [128, 2, 3, 128], BF16)
            wukT = sa("wukT", [128, 2, 128], BF16)
            wuv_pad = sa("wuvp", [128, 4, 128], BF16)
            wuv_tmp = sa("wuvt", [128, 256], BF16)
            wuk_tmp = sa("wukt", [128, 256], F32)
            vec = sa("vec3", [128, NV], F32)
            kvrow = sa("kvrow", [1, 128], F32)
            cmax2 = sa("cmax2", [1, 1], F32)
            for k in range(2):
                p.dma("pool", wqup[:, k, :], wqup_d[l, k * 128:(k + 1) * 128, :], (), (BW,))
                p.dma("pool", wqidx[:, k, :], wqidx_d[l, k * 128:(k + 1) * 128, :], (), (BW,))
            p.dma("pool", wuv_tmp[:], wuv_d[l, :, :], (), (BW,))
            p.dma("sp", wuk_tmp[:], wuk_d[l, :, :], (), (BW,))
            p.dma("sp", vec[:], vec_d[l, :, :], (), (BW,))
            p.dma("sp", kvrow[:], kvrow_d[l:l + 1, :], (), (BW,))
            mset("dve", wuv_pad[:], 0.0, (BW,))
            mset("dve", wqidx3[:], 0.0, (BW,))
            for h in range(8):
                cp("act", wqidx3[:, :, h // 3, (h % 3) * 32:(h % 3) * 32 + 32], wqidx[:, :, h * 32:(h + 1) * 32],
                   (BW,), (BW,))
            for j in range(4):
                cp("act", w_ik4[:, :, j * 32:(j + 1) * 32], wd[:, :, 384:416], (BW,), (BW,))
            for h in range(4):
                c0 = (h % 2) * 64
                cp("act", wuv_pad[:, h, c0:c0 + 64], wuv_tmp[:, h * 64:(h + 1) * 64], (BW,), (BW,))
            for hp in range(2):
                pt, pb = ps_pool.next()
                tr(pt[:, 0:128], wuk_tmp[:, hp * 128:(hp + 1) * 128], ident_f[:], (BW, B_const), (pb,))
                ts(wukT[:, hp, :], pt[:, 0:128], 0.125, None, ALU.mult, None, (pb,), (BW,))
            p.op("dve", lambda E: E.tensor_reduce(out=cmax2[:], in_=kvrow[:], axis=mybir.AxisListType.X, op=ALU.max,
                                                  apply_absolute_value=True), (BW,), (BW,))
            tt(cmax2[:], cmax2[:], cmax2[:], ALU.mult, (BW,), (BW,))
            ts(cmax2[:], cmax2[:], 128.0, None, ALU.mult, None, (BW,), (BW,))

            T32 = TPool(p, "p3t32_%d" % l, [128, 512], F32, 6)
            T16 = TPool(p, "p3t16_%d" % l, [128, 512], BF16, 4)
            hT_pool = TPool(p, "p3h_%d" % l, [128, KD, TB], BF16, 1)
            yo_pool = TPool(p, "p3y_%d" % l, [128, 2, TB], BF16, 2)
            ckvT = sa("ckvT", [128, S], BF16); B_ckvT = [Buf() for _ in range(NBLK)]
            ckv_tok = sa("ckv_tok", [128, NQT, 128], BF16); B_ckt = [Buf() for _ in range(NBLK)]
            kidx4 = sa("kidx4", [128, S], BF16); B_kidx = [Buf() for _ in range(NBLK)]
            qlatT = sa("qlatT", [128, 2, TB], BF16); B_ql = Buf()
            qsT = sa("qsT", [128, 3, TB], BF16); B_qs = Buf()
            qTsb = sa("qTsb", [128, 2, TB], BF16); B_qT = Buf()
            qaT2 = [sa("qaT%d" % i, [128, 4, TB], BF16) for i in range(2)]; B_qa2 = [Buf(), Buf()]
            iwT = sa("iwT", [8, 2, TB], F32); B_iw = Buf()
            sgn_tok = sa("sgn_tok", [128, 4, 8], F32); B_sg = Buf()
            dg2 = [sa("dg%d" % i, [128, 8, 128], BF16) for i in range(2)]; B_dg2 = [Buf(), Buf()]
            mrow2 = [sa("mrow%d" % i, [1, 4, TB], BF16) for i in range(2)]; B_mr2 = [Buf(), Buf()]
            mrowf = sa("mrowf", [1, TB], F32); B_mrf = Buf()
            sc2 = [sa("sc%d" % i, [128, S], F32) for i in range(2)]; B_sc2 = [Buf(), Buf()]
            negm2 = [sa("negm%d" % i, [128, S], BF16) for i in range(2)]; B_nm2 = [Buf(), Buf()]
            rh_pool = TPool(p, "rh_%d" % l, [128, 512], BF16, 10)
            pT_pool = TPool(p, "pT_%d" % l, [128, 512], BF16, 3)
            bis2 = [sa("bis%d" % i, [128, 8], F32) for i in range(2)]; B_bis2 = [Buf(), Buf()]
            dcols2 = [sa("dcols%d" % i, [128, NIT + 1], F32) for i in range(2)]
            pow2 = sa("pow2", [128, NIT + 1], F32)
            olat = sa("olat", [128, 4, 128], BF16); B_ol = Buf()
            ya2 = [sa("ya_sb%d" % i, [128, 2, TB], F32) for i in range(2)]; B_ya2 = [Buf(), Buf()]
            nd2 = [sa("nd%d" % i, [128, 2, 512], F32) for i in range(2)]; B_nd2 = [Buf(), Buf()]
            for k in range(NIT + 1):
                mset("dve", pow2[:, k:k + 1], 0.5 ** (k + 1), (BW,))

            def prep(s, blk):
                t0 = blk * TB
                par = blk % 2
                hT, hTB = hT_pool.next()
                p.dma("sp", hT[:], fm(hT_d, s, t0, TB), (), (hTB,))

                def proj(wt, c0, ncol):
                    pt, pb = ps_pool.next()
                    for k in range(KD):
                        mm(pt[0:ncol, :], wt[:, k, c0:c0 + ncol], hT[:, k, :], k == 0, k == KD - 1, (BW, hTB), (pb,))
                    return pt, pb

                aq = []
                for t in range(2):
                    pt, pb = proj(wd, t * 128, 128)
                    av, avB = T32.next()
                    cp("act", av[:], pt[:], (pb,), (avB,))
                    aq.append((av, avB))
                rs, rsB = rms_bcast(T16, T32, [(aq[t][0][:], aq[t][1]) for t in range(2)], 256)
                for t in range(2):
                    stt(qlatT[:, t, :], aq[t][0][:], vec[:, 40 + t:41 + t], rs[:], ALU.mult, ALU.mult,
                        (aq[t][1], BW, rsB), (B_ql,))
                pt, pb = proj(wd, 256, 128)
                av, avB = T32.next()
                cp("act", av[:], pt[:], (pb,), (avB,))
                rs, rsB = rms_bcast(T16, T32, [(av[:], avB)], 128)
                stt(ckvT[:, t0:t0 + TB], av[:], vec[:, 42:43], rs[:], ALU.mult, ALU.mult,
                    (avB, BW, rsB), (B_ckvT[blk],))
                for c in range(4):
                    pT_, pTB = ps_pool.next()
                    ptb = pT_[:].bitcast(BF16)
                    tr(ptb[:, 0:128], ckvT[:, t0 + c * 128:t0 + (c + 1) * 128], ident_b[:], (B_ckvT[blk], B_const), (pTB,))
                    cp("act", ckv_tok[:, blk * 4 + c, :], ptb[:, 0:128], (pTB,), (B_ckt[blk],))
                pt, pb = proj(w_ik4, 0, 128)
                cp("act", kidx4[:, t0:t0 + TB], pt[:], (pb,), (B_kidx[blk],))
                pw, pwB = proj(wd, 416, 8)
                ts(iwT[:, 0, :], pw[0:8, :], 8.0 ** -0.5, None, ALU.mult, None, (pwB,), (B_iw,))
                act(iwT[:, 1, :], iwT[:, 0, :], AF.Abs, (B_iw,), (B_iw,))
                for c in range(4):
                    pt, pb = ps_pool.next()
                    mm(pt[:, 0:8], iwT[:, 0, c * 128:(c + 1) * 128], ident_f[0:8, 0:8], True, True,
                       (B_iw, B_const), (pb,))
                    act(sgn_tok[:, c, :], pt[:, 0:8], AF.Sign, (pb,), (B_sg,))
                for t in range(3):
                    pq, pqB = ps_pool.next()
                    for k in range(2):
                        mm(pq[:], wqidx3[:, k, t, :], qlatT[:, k, :], k == 0, k == 1, (BW, B_ql), (pqB,))
                    pwb, pwbB = ps_pool.next()
                    mm(pwb[:], sel8[:, t, :], iwT[:, 1, :], True, True, (B_const, B_iw), (pwbB,))
                    wab, wabB = T32.next()
                    act(wab[:], pwb[:], AF.Copy, (pwbB,), (wabB,), scale=32.0 ** -0.5)
                    tt(qsT[:, t, :], pq[:], wab[:], ALU.mult, (pqB, wabB), (B_qs,))
                for t in range(2):
                    pq, pqB = ps_pool.next()
                    for k in range(2):
                        mm(pq[:], wqup[:, k, t * 128:(t + 1) * 128], qlatT[:, k, :], k == 0, k == 1, (BW, B_ql), (pqB,))
                    cp("act", qTsb[:, t, :], pq[:], (pqB,), (B_qT,))
                qaT, B_qa, mrow, B_mr = qaT2[par], B_qa2[par], mrow2[par], B_mr2[par]
                for h in range(4):
                    r0 = (h % 2) * 64
                    pa, paB = ps_pool.next()
                    mm(pa[:], wukT[r0:r0 + 64, h // 2, :], qTsb[r0:r0 + 64, h // 2, :], True, True, (BW, B_qT), (paB,))
                    cp("act", qaT[:, h, :], pa[:], (paB,), (B_qa,))
                    sq, sqB = T16.next()
                    act(sq[:], pa[:], AF.Square, (paB,), (sqB,))
                    qn2, qn2B = ps_pool.next()
                    mm(qn2[0:1, :], ones_b[:, 0:1], sq[:], True, True, (sqB, B_const), (qn2B,))
                    ts(mrowf[:], qn2[0:1, :], cmax2[:, 0:1], None, ALU.mult, None, (qn2B, BW), (B_mrf,))
                    act(mrowf[:], mrowf[:], AF.Sqrt, (B_mrf,), (B_mrf,))
                    ts(mrowf[:], mrowf[:], bmax[:, 0:1], -1.0, ALU.add, ALU.mult, (B_mrf, B_const), (B_mrf,))
                    ts(mrow[:, h, :], mrowf[:], relb_row[:, 124 + h:125 + h], None, ALU.add, None,
                       (B_mrf, B_const), (B_mr,))

            def geom(u):
                s, blk, c = u
                qi = blk * 4 + c
                nk = (qi + 1) * 128
                return qi, nk, (nk + 511) // 512, slice(c * 128, (c + 1) * 128), nk > TOPK

            def idx(u, ui):
                s, blk, c = u
                qi, nk, nch, qcs, use_topk = geom(u)
                if not use_topk:
                    return
                dg, B_dg = dg2[ui % 2], B_dg2[ui % 2]
                sc, B_sc = sc2[ui % 2], B_sc2[ui % 2]
                for hh in range(8):
                    ts(dg[:, hh, :], ident_f[:], sgn_tok[:, c, hh:hh + 1], None, ALU.mult, None,
                       (B_const, B_sg), (B_dg,), eng="pool")
                for ch in range(nch):
                    w = min(512, nk - ch * 512)
                    ks = slice(ch * 512, ch * 512 + w)
                    rhs_ = []
                    for hh in range(8):
                        r0 = (hh % 3) * 32
                        pi2, pi2B = ps_pool.next()
                        mm(pi2[:, 0:w], qsT[r0:r0 + 32, hh // 3, qcs], kidx4[r0:r0 + 32, ks], True, True,
                           (B_qs, B_kidx[ch]), (pi2B,))
                        rh, rhB = rh_pool.next()
                        act(rh[:, 0:w], pi2[:, 0:w], AF.Relu, (pi2B,), (rhB,))
                        rhs_.append((rh, rhB))
                    pacc, paccB = ps_pool.next()
                    for hh in range(8):
                        mm(pacc[:, 0:w], dg[:, hh, :], rhs_[hh][0][:, 0:w], hh == 0, hh == 7,
                           (B_dg, rhs_[hh][1]), (paccB,))
                    cp("act", sc[:, ks], pacc[:, 0:w], (paccB,), (B_sc,))

            def bisect(u, ui):
                s, blk, c = u
                qi, nk, nch, qcs, use_topk = geom(u)
                if not use_topk:
                    return
                negm, B_nm = negm2[ui % 2], B_nm2[ui % 2]
                sc, B_sc = sc2[ui % 2], B_sc2[ui % 2]
                bis, B_bis, dcols = bis2[ui % 2], B_bis2[ui % 2], dcols2[ui % 2]
                on_act = False
                p.op("dve", lambda E: E.tensor_reduce(out=bis[:, 0:1], in_=sc[:, 0:nk], axis=mybir.AxisListType.X,
                                                      op=ALU.min), (B_sc,), (B_bis,))
                tt(sc[:, nk - 128:nk], sc[:, nk - 128:nk], negts[:], ALU.add, (B_sc, B_const), (B_sc,))
                p.op("dve", lambda E: E.tensor_reduce(out=bis[:, 1:2], in_=sc[:, 0:nk], axis=mybir.AxisListType.X,
                                                      op=ALU.max), (B_sc,), (B_bis,))
                tt(bis[:, 2:3], bis[:, 1:2], bis[:, 0:1], ALU.subtract, (B_bis,), (B_bis,))
                ts(dcols[:], pow2[:], bis[:, 2:3], None, ALU.mult, None, (B_bis, BW), (B_bis,))
                tt(bis[:, 3:4], bis[:, 0:1], dcols[:, 0:1], ALU.add, (B_bis,), (B_bis,))
                if not on_act:
                    for k in range(NIT):
                        ts(negm[:, 0:nk], sc[:, 0:nk], bis[:, 3:4], 0.0, ALU.is_ge, ALU.add,
                           (B_sc, B_bis), (B_nm, B_bis), accum_out=bis[:, 4:5])
                        stt(bis[:, 5:6], bis[:, 4:5], float(TOPK) - 0.5, dcols[:, k:k + 1], ALU.is_ge, ALU.mult,
                            (B_bis,), (B_bis,))
                        stt(bis[:, 3:4], bis[:, 3:4], dcols[:, k + 1:k + 2], bis[:, 5:6], ALU.subtract, ALU.add,
                            (B_bis,), (B_bis,))
                else:
                    ts(bis[:, 7:8], bis[:, 3:4], -1.0, None, ALU.mult, None, (B_bis,), (B_bis,))
                    ts(dcols[:], dcols[:], -1.0, None, ALU.mult, None, (B_bis,), (B_bis,))
                    for k in range(NIT):
                        act(negm[:, 0:nk], sc[:, 0:nk], AF.Sign, (B_sc, B_bis), (B_nm, B_bis),
                            bias=bis[:, 7:8], scale=1.0, accum_out=bis[:, 4:5])
                        act(bis[:, 5:6], bis[:, 4:5], AF.Sign, (B_bis,), (B_bis,), scale=1.0,
                            bias=float(nk - 2 * TOPK + 1))
                        act(bis[:, 7:8], bis[:, 5:6], AF.Identity, (B_bis,), (B_bis,),
                            scale=dcols[:, k + 1:k + 2], bias=bis[:, 7:8])
                    ts(bis[:, 3:4], bis[:, 7:8], -1.0, None, ALU.mult, None, (B_bis,), (B_bis,))
                    ts(dcols[:, NIT:NIT + 1], dcols[:, NIT:NIT + 1], -1.0, None, ALU.mult, None, (B_bis,), (B_bis,))
                tt(bis[:, 6:7], bis[:, 3:4], dcols[:, NIT:NIT + 1], ALU.subtract, (B_bis,), (B_bis,))
                ts(negm[:, 0:nk], sc[:, 0:nk], bis[:, 6:7], -1.0, ALU.is_ge, ALU.add, (B_sc, B_bis), (B_nm,))
                if dbg and l == 0 and s == 0 and c == 3 and blk == NBLK - 1:
                    p.dma("sp", dbg_d["d_sc"][:, 0:nk], sc[:, 0:nk], (B_sc,), ())
                    p.dma("sp", dbg_d["d_thr"][:, 0:4], bis[:, 3:7], (B_bis,), ())

            def att(u, ui):
                s, blk, c = u
                qi, nk, nch, qcs, use_topk = geom(u)
                par = blk % 2
                qaT, B_qa, mrow, B_mr = qaT2[par], B_qa2[par], mrow2[par], B_mr2[par]
                negm, B_nm = negm2[ui % 2], B_nm2[ui % 2]
                pnum, pnumB = acc_pool.next()
                pden, pdenB = acc_pool.next()

                def logits(sj):
                    ss = slice(sj * 128, (sj + 1) * 128)
                    near = (qi - sj) <= 1
                    pl, plB = ps_pool.next()
                    pl3 = pl[:].rearrange("p (h t) -> p h t", h=4)
                    mm(pl3, ckvT[:, ss], qaT[:, :, qcs], True, False, (B_ckvT[sj // 4], B_qa), (plB,))
                    if near:
                        BT = BT0 if qi == sj else BT1
                        mm(pl[:], ident_b[:], BT[:].rearrange("p h t -> p (h t)"), False, False, (B_const,), (plB,))
                    if use_topk:
                        mm(pl[:], negm[:, ss], identx4[:].rearrange("p h t -> p (h t)"), False, False,
                           (B_nm, B_const), (plB,))
                    mm(pl3, ones_b[0:1, 0:128], mrow[:, :, qcs], False, True, (B_const, B_mr), (plB,))
                    return pl, plB

                cur = logits(0)
                for sj in range(qi + 1):
                    pl, plB = cur
                    pT, pTB2 = pT_pool.next()
                    act(pT[:], pl[:], AF.Exp, (plB,), (pTB2,))
                    if sj + 1 <= qi:
                        cur = logits(sj + 1)
                    mm(pnum[:], ckv_tok[:, sj, :], pT[:], sj == 0, sj == qi, (B_ckt[sj // 4], pTB2), (pnumB,))
                    mm(pden[:], ones_b[:], pT[:], sj == 0, sj == qi, (B_const, pTB2), (pdenB,))
                nd, ndB = nd2[ui % 2], B_nd2[ui % 2]
                cp("act", nd[:, 0, :], pnum[:], (pnumB,), (ndB,))
                act(nd[:, 1, :], pden[:], AF.Ln, (pdenB,), (ndB,))
                act(nd[:, 1, :], nd[:, 1, :], AF.Exp, (ndB,), (ndB,), scale=-1.0)

            def epi(u, ui):
                s, blk, c = u
                qi, nk, nch, qcs, use_topk = geom(u)
                par = blk % 2
                ya_sb, B_ya = ya2[par], B_ya2[par]
                nd, ndB = nd2[ui % 2], B_nd2[ui % 2]
                tt(olat[:].rearrange("p h t -> p (h t)"), nd[:, 0, :], nd[:, 1, :], ALU.mult, (ndB,), (B_ol,))
                for t in range(2):
                    py_, pyB_ = ps_pool.next()
                    for hh in range(2):
                        h = t * 2 + hh
                        mm(py_[:, 0:128], wuv_pad[:, h, :], olat[:, h, :], hh == 0, hh == 1, (BW, B_ol), (pyB_,))
                    cp("act", ya_sb[:, t, qcs], py_[:, 0:128], (pyB_,), (B_ya,))
                if c == 3:
                    finish(s, blk)

            def finish(s, blk):
                t0 = blk * TB
                par = blk % 2
                ya_sb, B_ya = ya2[par], B_ya2[par]
                yo, yoB = yo_pool.next()
                rs, rsB = rms_bcast(T16, T32, [(ya_sb[:, t, :], B_ya) for t in range(2)], 256)
                for t in range(2):
                    stt(yo[:, t, :], ya_sb[:, t, :], vec[:, 22 + t:23 + t], rs[:], ALU.mult, ALU.mult,
                        (B_ya, BW, rsB), (yoB,))
                    if dbg_on(s, blk):
                        tmp, tmpB = T32.next()
                        cp("dve", tmp[:], yo[:, t, :], (yoB,), (tmpB,))
                        p.dma("sp", dbg_d["d_ya"][t * 128:(t + 1) * 128, :], tmp[:], (tmpB,), ())
                p.dma("sp", yT_d[s, blk, :, 6:8, :], yo[:], (yoB,), ())

            units = [(s, blk, c) for s in range(NSEQ) for blk in range(NBLK) for c in range(4)]
            nu = len(units)
            prep(units[0][0], units[0][1])
            idx(units[0], 0)
            for ui, u in enumerate(units):
                if ui + 1 < nu:
                    nx = units[ui + 1]
                    if nx[2] == 0:
                        prep(nx[0], nx[1])
                    idx(nx, ui + 1)
                bisect(u, ui)
                if ui > 0:
                    att(units[ui - 1], ui - 1)
                if ui > 1:
                    epi(units[ui - 2], ui - 2)
            att(units[-1], nu - 1)
            if nu > 1:
                epi(units[-2], nu - 2)
            epi(units[-1], nu - 1)
            p.barrier()

        with ExitStack() as esA:
            p.cur_es = esA
            BW = Buf()
            w_o_sb = esA.enter_context(nc.sbuf_tensor("w_o_%d" % l, [128, KD, D], BF16))
            for k in range(KD):
                p.dma("pool", w_o_sb[:, k, :], w_o_d[l, k * 128:(k + 1) * 128, :], (), (BW,))
            xblk_pool = TPool(p, "p4x_%d" % l, [128, KD, TB], F32, 2)
            yT_pool = TPool(p, "p4y_%d" % l, [128, KD, TB], BF16, 2)
            for s in range(NSEQ):
                for blk in range(NBLK):
                    t0 = blk * TB
                    xb, xbB = xblk_pool.next()
                    p.dma("sp", xb[:], fm(xs_d, s, t0, TB), (), (xbB,))
                    yT, yTB = yT_pool.next()
                    p.dma("sp", yT[:], fm(yT_d, s, t0, TB), (), (yTB,))
                    for m in range(KD):
                        po, poB = ps_pool.next()
                        for k in range(KD):
                            mm(po[:], w_o_sb[:, k, m * 128:(m + 1) * 128], yT[:, k, :], k == 0, k == KD - 1,
                               (BW, yTB), (poB,))
                        stt(xb[:, m, :], po[:], modT[:, l, 16 + m, s:s + 1], xb[:, m, :], ALU.mult, ALU.add,
                            (poB, B_mod, xbB), (xbB,))
                    p.dma("sp", fm(xs_d, s, t0, TB), xb[:], (xbB,), ())
                    if dbg_on(s, blk):
                        p.dma("sp", dbg_d["d_x1"].rearrange("(k p) n -> p k n", p=128), xb[:], (xbB,), ())
            p.barrier()

        with ExitStack() as esB:
            p.cur_es = esB
            def sbb(name, shape, dt=F32):
                return esB.enter_context(nc.sbuf_tensor("%s_%d" % (name, l), shape, dt))

            BW = Buf()
            TBM = 256
            w1 = sbb("w1", [128, KD, 4 * D], BF16)
            w2 = sbb("w2", [128, 32, D], BF16)
            vec = sbb("vecB", [128, NV], F32)
            p.dma("sp", vec[:], vec_d[l, :, :], (), (BW,))
            for k in range(KD):
                p.dma("pool", w1[:, k, :], w1_d[l, k * 128:(k + 1) * 128, :], (), (BW,))
            for k in range(32):
                p.dma("pool", w2[:, k, :], w2_d[l, k * 128:(k + 1) * 128, :], (), (BW,))
            xq_pool = TPool(p, "xq_%d" % l, [128, KD, TBM], F32, 2)
            h2_pool = TPool(p, "h2_%d" % l, [128, KD, TBM], BF16, 2)
            aT_pool = TPool(p, "aT_%d" % l, [128, 32, TBM], BF16, 1)
            M32 = TPool(p, "m32_%d" % l, [128, TBM], F32, 4)
            RS5 = TPool(p, "p5rs_%d" % l, [128, TBM], F32, 2)
            M16 = TPool(p, "m16_%d" % l, [128, TBM], BF16, 4)
            for s in range(NSEQ):
                A2w, A2wB = M32.next()
                tt(A2w[:, 0:8], modT[:, l, 32:40, s], vec[:, 8:16], ALU.mult, (B_mod, BW), (A2wB,))
                A2s = sbb("A2s_%d" % s, [128, 8], F32)
                cp("dve", A2s[:], A2w[:, 0:8], (A2wB,), (BW,))
                for blk in range(S // TBM):
                    t0 = blk * TBM
                    xb, xbB = xq_pool.next()
                    p.dma("sp", xb[:], fm(xs_d, s, t0, TBM), (), (xbB,))
                    rstd, rstdB = rms_bcast(M16, RS5, [(xb[:, k, :], xbB) for k in range(KD)], D, width=TBM)
                    h2, h2B = h2_pool.next()
                    for k in range(KD):
                        tmp, tmpB = M32.next()
                        tt(tmp[:], xb[:, k, :], rstd[:], ALU.mult, (xbB, rstdB), (tmpB,))
                        act(h2[:, k, :], tmp[:], AF.Identity, (tmpB, BW, B_mod), (h2B,),
                            scale=A2s[:, k:k + 1], bias=modT[:, l, 24 + k, s:s + 1])
                    aT, aTB = aT_pool.next()
                    for m in range(32):
                        pm, pmB = ps_pool.next()
                        for k in range(KD):
                            mm(pm[:, 0:TBM], w1[:, k, m * 128:(m + 1) * 128], h2[:, k, :], k == 0, k == KD - 1,
                               (BW, h2B), (pmB,))
                        rl, rlB = M32.next()
                        act(rl[:], pm[:, 0:TBM], AF.Relu, (pmB,), (rlB,))
                        tt(aT[:, m, :], rl[:], rl[:], ALU.mult, (rlB,), (aTB,))
                    for m in range(KD):
                        po, poB = ps_pool.next()
                        for k in range(32):
                            mm(po[:, 0:TBM], w2[:, k, m * 128:(m + 1) * 128], aT[:, k, :], k == 0, k == 31,
                               (BW, aTB), (poB,))
                        stt(xb[:, m, :], po[:, 0:TBM], modT[:, l, 40 + m, s:s + 1], xb[:, m, :], ALU.mult, ALU.add,
                            (poB, B_mod, xbB), (xbB,))
                    p.dma("sp", fm(xs_d, s, t0, TBM), xb[:], (xbB,), ())
                    if dbg and l == 0 and s == 0 and blk // 2 == want_dbg_blk:
                        p.dma("sp", dbg_d["d_x2"].rearrange("(k p) n -> p k n", p=128)[:, :, (blk % 2) * TBM:(blk % 2 + 1) * TBM],
                              xb[:], (xbB,), ())
            p.barrier()

    with ExitStack() as es2:
        p.cur_es = es2
        xq_pool = TPool(p, "fx", [128, KD, 128], F32, 2)
        fo_pool = TPool(p, "fo", [128, D], F32, 2)
        F32p = TPool(p, "f32p", [128, 128], F32, 6)
        RSF = TPool(p, "frs", [128, 128], F32, 2)
        F16p = TPool(p, "f16p", [128, 128], BF16, 4)
        for s in range(NSEQ):
            for tq in range(NQT):
                xb, xbB = xq_pool.next()
                p.dma("sp", xb[:], xs_d[s, tq // 4, :, :, (tq % 4) * 128:(tq % 4 + 1) * 128], (), (xbB,))
                pss, pssB = ps_pool.next()
                for k in range(KD):
                    sq, sqB = F16p.next()
                    act(sq[:], xb[:, k, :], AF.Square, (xbB,), (sqB,))
                    mm(pss[:, 0:128], ones_b[:], sq[:], k == 0, k == KD - 1, (sqB, B_const), (pssB,))
                rstd, rstdB = RSF.next()
                act(rstd[:], pss[:, 0:128], AF.Sqrt, (pssB,), (rstdB,), scale=1.0 / D, bias=EPS)
                p.op("dve", lambda E: E.reciprocal(out=rstd[:], in_=rstd[:]), (rstdB,), (rstdB,))
                fo, foB = fo_pool.next()
                for half in range(2):
                    pt, pb = ps_pool.next()
                    for kk in range(4):
                        k = half * 4 + kk
                        tmp, tmpB = F32p.next()
                        stt(tmp[:], xb[:, k, :], fnw[:, k:k + 1], rstd[:], ALU.mult, ALU.mult,
                            (xbB, B_const, rstdB), (tmpB,))
                        tr(pt[:, kk * 128:(kk + 1) * 128], tmp[:], ident_f[:], (tmpB, B_const), (pb,))
                    cp("act" if half == 0 else "dve", fo[:, half * 512:(half + 1) * 512], pt[:], (pb,), (foB,))
                p.dma("sp", out_d[s, tq * 128:(tq + 1) * 128, :], fo[:], (foB,), ())
        p.barrier()
    es.close()
    return nc, p


def pack_inputs(inputs, DEPTH, NSEQ, ncores):
    f = lambda a: np.ascontiguousarray(np.asarray(a, dtype=np.float32))
    x = f(inputs["x"]); c = f(inputs["c"])
    S = x.shape[1]

    def fmv(v):
        v = f(v)
        return v.reshape(-1, 128).T

    vecs = np.zeros((DEPTH, 128, NV), np.float32)
    for l in range(DEPTH):
        v = vecs[l]
        v[:, 0:8] = fmv(inputs["norm1_w"][l])
        v[:, 8:16] = fmv(inputs["norm2_w"][l])
        v[:, 16:24] = fmv(inputs["group_norm_w"][l])
        cw = f(inputs["conv_w"][l])
        for t in range(2):
            for k in range(4):
                v[:, 24 + t * 4 + k] = cw[k, t * 128:(t + 1) * 128]
        v[:, 32:34] = fmv(inputs["conv_b"][l])
        v[:, 34:36] = fmv(f(inputs["lru_ba"][l]).reshape(-1))
        v[:, 36:38] = fmv(f(inputs["lru_bx"][l]).reshape(-1))
        v[:, 38:40] = fmv(inputs["lru_lambda"][l])
        v[:, 40:42] = fmv(inputs["q_lat_norm_w"][l])
        v[:, 42:43] = fmv(inputs["kv_lat_norm_w"][l])
        v[:, 43:91] = fmv(inputs["b_ada"][l])
        v[0:4, 91] = f(inputs["mlstm_bi"][l])
        v[0:4, 92] = f(inputs["mlstm_bf"][l])
    shared = {
        "w_in": f(inputs["w_in"])[:DEPTH], "w_o": f(inputs["w_o"])[:DEPTH], "w_ada": f(inputs["w_ada"])[:DEPTH],
        "w_mlp1": f(inputs["w_mlp1"])[:DEPTH], "w_mlp2": f(inputs["w_mlp2"])[:DEPTH],
        "lru_wa": f(inputs["lru_wa"])[:DEPTH], "lru_wx": f(inputs["lru_wx"])[:DEPTH],
        "w_q_up": f(inputs["w_q_up"])[:DEPTH].reshape(DEPTH, 256, 256),
        "w_qidx_up": f(inputs["w_qidx_up"])[:DEPTH].reshape(DEPTH, 256, 256),
        "w_uk": f(inputs["w_uk"])[:DEPTH].reshape(DEPTH, 128, 256),
        "w_uv": f(inputs["w_uv"])[:DEPTH].reshape(DEPTH, 128, 256),
        "vec": vecs,
        "gmrow": np.ascontiguousarray(f(inputs["group_norm_w"])[:DEPTH, 256:768]),
        "kvrow": f(inputs["kv_lat_norm_w"])[:DEPTH],
        "relb": f(inputs["rel_bias"]).reshape(128),
        "fnw": np.ascontiguousarray(fmv(inputs["final_norm_w"])),
    }
    for k, v in make_consts().items():
        shared["c_" + k] = v
    in_maps = []
    for ci in range(ncores):
        d = dict(shared)
        d["x"] = np.ascontiguousarray(x[ci * NSEQ:(ci + 1) * NSEQ])
        cs = c[ci * NSEQ:(ci + 1) * NSEQ]
        d["cT"] = np.ascontiguousarray(cs.T.reshape(KD, 128, NSEQ).transpose(1, 0, 2))
        in_maps.append(d)
    return in_maps


_CACHE = {}


def kernel(**inputs):
    x = np.asarray(inputs["x"])
    B, S, _ = x.shape
    ncores = 8
    NSEQ = B // ncores
    key = (S, DEPTH_FULL, NSEQ)
    if key not in _CACHE:
        _CACHE[key] = build(S, DEPTH_FULL, NSEQ)[0]
    nc = _CACHE[key]
    in_maps = pack_inputs(inputs, DEPTH_FULL, NSEQ, ncores)
    res = run_bass_kernel_spmd(nc, in_maps, core_ids=list(range(ncores)))
    out = np.concatenate([np.asarray(r["out"]) for r in res.results], axis=0)
    return out.astype(np.float32)
```
